# Optimizing a Trainium2 kernel written in Bass

```python
import math
import jax, jax.numpy as jnp
from jax import lax
import numpy as np

D_MODEL = 1024
BATCH = 16
SEQ = 4096
DEPTH = 4

N_MIXERS = 2
N_ATTN_LAYERS = (DEPTH + 1) // 2
N_CONV_LAYERS = DEPTH // 2
N_HEADS = 8
HEAD_DIM = D_MODEL // N_HEADS // 2
V_DIM = 2 * HEAD_DIM
Q_BLOCK = 128
NUM_BUCKETS = 32
MAX_DISTANCE = 128
CONV_WIDTH = 3
N_EXPERTS = 32
TOP_K = 4
D_EXPERT = D_MODEL
SWIGLU_ALPHA = 1.702
SWIGLU_LIMIT = 7.0
MOE_BLOCK = 512
DEEPNORM_ALPHA = (2.0 * DEPTH) ** 0.25
DEEPNORM_BETA = (8.0 * DEPTH) ** -0.25
LN_EPS = 1e-5

kernel_name = "hybrid_diffattn_shortconv_moe_encoder"


def layer_norm(x, g, b):
    xf = x.astype(jnp.float32)
    mu = jnp.mean(xf, axis=-1, keepdims=True)
    var = jnp.mean(jnp.square(xf - mu), axis=-1, keepdims=True)
    y = (xf - mu) * lax.rsqrt(var + LN_EPS) * g.astype(jnp.float32) + b.astype(jnp.float32)
    return y.astype(x.dtype)


def rms_norm(x, g):
    xf = x.astype(jnp.float32)
    y = xf * lax.rsqrt(jnp.mean(jnp.square(xf), axis=-1, keepdims=True) + LN_EPS) * g.astype(jnp.float32)
    return y.astype(x.dtype)


def rel_bucket(rel):
    nb = NUM_BUCKETS // 2
    max_exact = nb // 2
    ret = jnp.where(rel > 0, nb, 0)
    n = jnp.abs(rel)
    nf = jnp.maximum(n, 1).astype(jnp.float32)
    large = max_exact + (jnp.log(nf / max_exact) / math.log(MAX_DISTANCE / max_exact)
                         * (nb - max_exact)).astype(jnp.int32)
    large = jnp.minimum(large, nb - 1)
    return ret + jnp.where(n < max_exact, n, large)


def diff_attention(x, w_in, w_out, lam_p, subln_g, rel_bias, lambda_init):
    B, S, D = x.shape
    qkv = x @ w_in
    q, k, v = jnp.split(qkv, 3, axis=-1)
    q = q.reshape(B, S, N_HEADS, 2, HEAD_DIM) * (HEAD_DIM ** -0.5)
    k = k.reshape(B, S, N_HEADS, 2, HEAD_DIM)
    v = v.reshape(B, S, N_HEADS, V_DIM)
    lf = lam_p.astype(jnp.float32)
    lam = jnp.exp(jnp.sum(lf[0] * lf[1])) - jnp.exp(jnp.sum(lf[2] * lf[3])) + lambda_init
    nqb = S // Q_BLOCK
    qb = q.reshape(B, nqb, Q_BLOCK, N_HEADS, 2, HEAD_DIM).transpose(1, 0, 2, 3, 4, 5)
    k_pos = jnp.arange(S, dtype=jnp.int32)

    def block(args):
        qblk, start = args
        q_pos = start + jnp.arange(Q_BLOCK, dtype=jnp.int32)
        bucket = rel_bucket(k_pos[None, :] - q_pos[:, None])
        bias = rel_bias[bucket].astype(jnp.float32).transpose(2, 0, 1)
        s = jnp.einsum('bqhmd,bkhmd->bmhqk', qblk, k).astype(jnp.float32) + bias
        a = jax.nn.softmax(s, axis=-1)
        diff = a[:, 0] - lam * a[:, 1]
        return jnp.einsum('bhqk,bkhv->bqhv', diff.astype(v.dtype), v)

    o = lax.map(block, (qb, jnp.arange(nqb, dtype=jnp.int32) * Q_BLOCK))
    o = o.transpose(1, 0, 2, 3, 4).reshape(B, S, N_HEADS, V_DIM)
    o = rms_norm(o, subln_g) * (1.0 - lambda_init)
    return o.reshape(B, S, N_HEADS * V_DIM) @ w_out


def short_conv(x, w_in, conv_w, w_out):
    B, S, D = x.shape
    h = x @ w_in
    gate_b, gate_c, u = jnp.split(h, 3, axis=-1)
    z = gate_c * u
    zc = lax.conv_general_dilated(z, conv_w.reshape(CONV_WIDTH, 1, D).astype(z.dtype),
                                  window_strides=(1,), padding=((CONV_WIDTH // 2, CONV_WIDTH // 2),),
                                  dimension_numbers=('NWC', 'WIO', 'NWC'),
                                  feature_group_count=D)
    return (gate_b * zc) @ w_out


def clamped_swiglu(h):
    glu = jnp.minimum(h[..., ::2], SWIGLU_LIMIT)
    lin = jnp.clip(h[..., 1::2], -SWIGLU_LIMIT, SWIGLU_LIMIT)
    return glu * jax.nn.sigmoid(SWIGLU_ALPHA * glu) * (lin + 1.0)


def moe(x2d, router_w, router_b, w1, b1, w2, b2):
    N, D = x2d.shape
    NK = N * TOP_K
    logits = (x2d @ router_w).astype(jnp.float32) + router_b.astype(jnp.float32)
    top_v, top_e = lax.top_k(logits, TOP_K)
    gates = jax.nn.softmax(top_v, axis=-1)
    flat_e = top_e.reshape(-1).astype(jnp.int32)
    flat_tok = jnp.arange(NK, dtype=jnp.int32) // TOP_K
    flat_g = gates.reshape(-1)
    order = jnp.argsort(flat_e)
    se, st, sg = flat_e[order], flat_tok[order], flat_g[order]
    counts = jnp.bincount(flat_e, length=N_EXPERTS).astype(jnp.int32)
    padded = (counts + MOE_BLOCK - 1) // MOE_BLOCK * MOE_BLOCK
    g_start = jnp.cumsum(counts) - counts
    p_end = jnp.cumsum(padded)
    p_start = p_end - padded
    dest = p_start[se] + jnp.arange(NK, dtype=jnp.int32) - g_start[se]
    n_blocks = (NK + N_EXPERTS * (MOE_BLOCK - 1) + MOE_BLOCK - 1) // MOE_BLOCK
    P = n_blocks * MOE_BLOCK
    row_tok = jnp.zeros((P,), jnp.int32).at[dest].set(st)
    row_gate = jnp.zeros((P,), jnp.float32).at[dest].set(sg)
    blk_e = jnp.minimum(jnp.searchsorted(p_end, jnp.arange(n_blocks, dtype=jnp.int32) * MOE_BLOCK,
                                         side='right'), N_EXPERTS - 1)

    def expert_block(args):
        tok, g, e = args
        h = x2d[tok] @ w1[e] + b1[e]
        y = clamped_swiglu(h) @ w2[e] + b2[e]
        return y * g[:, None].astype(y.dtype)

    rows = lax.map(expert_block, (row_tok.reshape(n_blocks, MOE_BLOCK),
                                  row_gate.reshape(n_blocks, MOE_BLOCK), blk_e))
    return jnp.zeros_like(x2d).at[row_tok].add(rows.reshape(P, D).astype(x2d.dtype))


def setup_inputs(seed: int = 0) -> dict:
    key = jax.random.key(seed)
    ks = jax.random.split(key, 20)
    D, E, F = D_MODEL, N_EXPERTS, D_EXPERT
    nrm = jax.random.normal
    return {
        "x": nrm(ks[0], (BATCH, SEQ, D), jnp.float32),
        "rel_bias": 0.5 * nrm(ks[1], (NUM_BUCKETS, N_HEADS), jnp.float32),
        "attn_w_in": nrm(ks[2], (N_ATTN_LAYERS, D, 3 * D), jnp.float32) * D ** -0.5,
        "attn_lambda": 0.1 * nrm(ks[3], (N_ATTN_LAYERS, 4, HEAD_DIM), jnp.float32),
        "attn_subln": 1.0 + 0.01 * nrm(ks[4], (N_ATTN_LAYERS, V_DIM), jnp.float32),
        "attn_w_out": nrm(ks[5], (N_ATTN_LAYERS, N_HEADS * V_DIM, D), jnp.float32) * (N_HEADS * V_DIM) ** -0.5 * DEEPNORM_BETA,
        "conv_w_in": nrm(ks[6], (N_CONV_LAYERS, D, 3 * D), jnp.float32) * D ** -0.5,
        "conv_w": nrm(ks[7], (N_CONV_LAYERS, CONV_WIDTH, D), jnp.float32) * CONV_WIDTH ** -0.5,
        "conv_w_out": nrm(ks[8], (N_CONV_LAYERS, D, D), jnp.float32) * D ** -0.5 * DEEPNORM_BETA,
        "router_w": nrm(ks[9], (DEPTH, D, E), jnp.float32) * D ** -0.5,
        "router_b": 0.01 * nrm(ks[10], (DEPTH, E), jnp.float32),
        "w1": nrm(ks[11], (DEPTH, E, D, 2 * F), jnp.float32) * D ** -0.5,
        "b1": 0.01 * nrm(ks[12], (DEPTH, E, 2 * F), jnp.float32),
        "w2": nrm(ks[13], (DEPTH, E, F, D), jnp.float32) * F ** -0.5 * DEEPNORM_BETA,
        "b2": 0.01 * nrm(ks[14], (DEPTH, E, D), jnp.float32),
        "ln_g": 1.0 + 0.01 * nrm(ks[15], (DEPTH, 2, D), jnp.float32),
        "ln_b": 0.01 * nrm(ks[16], (DEPTH, 2, D), jnp.float32),
    }


def reference(x, rel_bias, attn_w_in, attn_lambda, attn_subln, attn_w_out,
              conv_w_in, conv_w, conv_w_out, router_w, router_b, w1, b1, w2, b2,
              ln_g, ln_b):
    B, S, D = x.shape
    for i in range(DEPTH):
        j = i // N_MIXERS
        if i % N_MIXERS == 0:
            lambda_init = 0.8 - 0.6 * math.exp(-0.3 * i)
            h = diff_attention(x, attn_w_in[j], attn_w_out[j], attn_lambda[j], attn_subln[j],
                               rel_bias, lambda_init)
        else:
            h = short_conv(x, conv_w_in[j], conv_w[j], conv_w_out[j])
        x = layer_norm(DEEPNORM_ALPHA * x + h, ln_g[i, 0], ln_b[i, 0])
        m = moe(x.reshape(B * S, D), router_w[i], router_b[i], w1[i], b1[i], w2[i], b2[i])
        x = layer_norm(DEEPNORM_ALPHA * x + m.reshape(B, S, D), ln_g[i, 1], ln_b[i, 1])
    return x
```

```python
import math
import numpy as np
import concourse.bass as bass
import concourse.mybir as mybir
from concourse.bass_utils import run_bass_kernel_spmd
from contextlib import ExitStack

F32 = mybir.dt.float32
BF16 = mybir.dt.bfloat16
I32 = mybir.dt.int32
AF = mybir.ActivationFunctionType
ALU = mybir.AluOpType
AX = mybir.AxisListType

D = 1024
NTOK = 8192
NT = NTOK // 128
SEQ = 4096
E = 32
BK = 128
NB = (NTOK * 4 + E * (BK - 1) + BK - 1) // BK
PROWS = NB * BK
BIG = 65536.0
BIGI = float(1 << 22)
ALPHA = 8.0 ** 0.25
LN_EPS = 1e-5
DEPTH = 4


class Buf:
    __slots__ = ("t", "w", "r")

    def __init__(self, t):
        self.t = t
        self.w = None
        self.r = {}

    def __getitem__(self, idx):
        return self.t[idx]


class Sched:
    def __init__(self, nc, es, ndma=(("sp", 16), ("pool", 8), ("act", 4), ("bg", 6))):
        self.nc = nc
        self.es = es
        self.eng = {"pe": nc.tensor, "act": nc.scalar, "dve": nc.vector,
                    "pool": nc.gpsimd, "sp": nc.sync}
        self.csem = {k: es.enter_context(nc.semaphore(f"c_{k}"))
                     for k in ("pe", "act", "dve", "pool")}
        self.ccnt = {k: 0 for k in self.csem}
        self.dsem = {q: [es.enter_context(nc.semaphore(f"d_{q}{i}")) for i in range(n)]
                     for q, n in ndma}
        self.dcnt = {q: [0] * len(v) for q, v in self.dsem.items()}
        self.dnext = {q: 0 for q in self.dsem}
        self.seen = {k: {} for k in self.eng}
        self.qeng = {"sp": "sp", "pool": "pool", "act": "act", "bg": "pool"}
        self.nwait = 0
        self.ninst = 0

    def _uid(self, name):
        self.uid = getattr(self, "uid", 0) + 1
        return f"{name}_{self.uid}"

    def sb(self, name, shape, dt):
        return Buf(self.es.enter_context(self.nc.sbuf_tensor(self._uid(name), list(shape), dt)))

    def ps(self, name, shape, dt=F32):
        return Buf(self.es.enter_context(self.nc.psum_tensor(self._uid(name), list(shape), dt)))

    def dram(self, name, shape, dt, kind="Internal"):
        return Buf(self.nc.dram_tensor(name, list(shape), dt, kind=kind))

    def _wait(self, e, ev):
        key, sem, val = ev
        if e == "pe" and key == "pe":
            return
        if self.seen[e].get(key, 0) < val:
            self.eng[e].wait_ge(sem, val)
            self.seen[e][key] = val
            self.nwait += 1

    def _deps(self, e, reads, writes):
        for b in reads:
            if b.w is not None:
                self._wait(e, b.w)
        for b in writes:
            if b.w is not None:
                self._wait(e, b.w)
            for ev in b.r.values():
                self._wait(e, ev)

    def _record(self, ev, reads, writes):
        for b in reads:
            b.r[ev[0]] = ev
        for b in writes:
            b.w = ev
            b.r = {}

    def op(self, e, fn, reads=(), writes=()):
        self._deps(e, reads, writes)
        inst = fn()
        self.ccnt[e] += 1
        inst.then_inc(self.csem[e], 1)
        self._record((e, self.csem[e], self.ccnt[e]), reads, writes)
        self.ninst += 1
        return inst

    def dma(self, q, out, in_, reads=(), writes=(), indirect=None, **kw):
        e = self.qeng[q]
        self._deps(e, reads, writes)
        i = self.dnext[q]
        self.dnext[q] = (i + 1) % len(self.dsem[q])
        sem = self.dsem[q][i]
        key = ("d", q, i)
        prev = self.dcnt[q][i]
        self._wait(e, (key, sem, prev))
        if indirect is not None:
            inst = self.eng[e].indirect_dma_start(out=out, in_=in_, **indirect, **kw)
        else:
            inst = self.eng[e].dma_start(out=out, in_=in_, **kw)
        inst.then_inc(sem, 16)
        self.dcnt[q][i] = prev + 16
        self._record((key, sem, prev + 16), reads, writes)
        self.ninst += 1
        return inst

    def wait_bg(self, engines=("pool",)):
        for i, sm in enumerate(self.dsem["bg"]):
            if self.dcnt["bg"][i] > 0:
                for e in engines:
                    self._wait(e, (("d", "bg", i), sm, self.dcnt["bg"][i]))

    def barrier(self):
        evs = [(k, self.csem[k], self.ccnt[k]) for k in self.csem if self.ccnt[k] > 0]
        for q in self.dsem:
            if q == "bg":
                continue
            for i, s in enumerate(self.dsem[q]):
                if self.dcnt[q][i] > 0:
                    evs.append((("d", q, i), s, self.dcnt[q][i]))
        for e in self.eng:
            for ev in evs:
                if ev[0] == e:
                    continue
                self._wait(e, ev)


def gather(ix_ap):
    return dict(out_offset=None, in_offset=bass.IndirectOffsetOnAxis(ap=ix_ap, axis=0))


def scatter(ix_ap):
    return dict(out_offset=bass.IndirectOffsetOnAxis(ap=ix_ap, axis=0), in_offset=None)


class K:
    def __init__(self, nc, es):
        self.nc = nc
        self.es = es
        self.S = Sched(nc, es)
        self.v = nc.vector
        self.g = nc.gpsimd
        self.a = nc.scalar
        self.pe = nc.tensor
        self.nl = DEPTH
        self.dbg = {}
        self.fuse_router = False

    def consts(self):
        S, nc = self.S, self.nc
        self.identf = S.sb("identf", [128, 128], F32)
        self.ident = S.sb("ident", [128, 128], BF16)
        self.onesb = S.sb("onesb", [128, 128], BF16)
        self.onesf = S.sb("onesf", [128, 128], F32)
        self.lsb = S.sb("lsb", [128, 128], BF16)
        tmp = S.sb("ctmp", [128, 128], F32)
        S.op("pool", lambda: self.g.memset(self.onesf[:], 1.0), writes=[self.onesf])
        S.op("pool", lambda: self.g.memset(self.identf[:], 1.0), writes=[self.identf])
        S.op("pool", lambda: self.g.affine_select(out=self.identf[:], in_=self.identf[:], pattern=[[-1, 128]],
                                                  compare_op=ALU.is_equal, fill=0.0, base=0, channel_multiplier=1),
             reads=[self.identf], writes=[self.identf])
        S.op("pool", lambda: self.g.memset(tmp[:], 1.0), writes=[tmp])
        S.op("pool", lambda: self.g.affine_select(out=tmp[:], in_=tmp[:], pattern=[[1, 128]],
                                                  compare_op=ALU.is_gt, fill=0.0, base=0, channel_multiplier=-1),
             reads=[tmp], writes=[tmp])
        S.op("dve", lambda: self.v.tensor_copy(out=self.ident[:], in_=self.identf[:]), reads=[self.identf], writes=[self.ident])
        S.op("dve", lambda: self.v.tensor_copy(out=self.onesb[:], in_=self.onesf[:]), reads=[self.onesf], writes=[self.onesb])
        self.inv128 = S.sb("inv128", [128, 128], BF16)
        S.op("dve", lambda: self.v.tensor_scalar(out=self.inv128[:], in0=self.onesf[:], scalar1=1.0 / 128.0, scalar2=None, op0=ALU.mult),
             reads=[self.onesf], writes=[self.inv128])
        S.op("dve", lambda: self.v.tensor_copy(out=self.lsb[:], in_=tmp[:]), reads=[tmp], writes=[self.lsb])
        self.kp = S.sb("kp", [128, 8], F32)
        S.op("pool", lambda: self.g.iota(out=self.kp[:], pattern=[[128, 8]], base=0, channel_multiplier=1,
                                         allow_small_or_imprecise_dtypes=True), writes=[self.kp])
        self.bvals = S.sb("bvals", [128, NB], F32)
        S.op("pool", lambda: self.g.iota(out=self.bvals[:], pattern=[[BK, NB]], base=0, channel_multiplier=0,
                                         allow_small_or_imprecise_dtypes=True), writes=[self.bvals])
        self.reg_wrow = nc.gpsimd.to_reg(E * 128 - 1)
        self.reg_brow = nc.gpsimd.to_reg(E - 1)
        self.reg_prow = nc.gpsimd.to_reg(PROWS - 1)
        self.mask_all = S.sb("mask_all", [128, NT, E], BF16)
        self.gates_all = S.sb("gates_all", [128, NT, E], F32)
        self.idx_all = S.sb("idx_all", [128, NT, 4], I32)
        self.gsel_all = S.sb("gsel_all", [128, NT, 4], F32)
        self.widx = S.sb("widx", [128, NB], I32)
        self.bidx = S.sb("bidx", [128, NB], I32)

    def ln_a(self, z, st, mv, rstd):
        S, v = self.S, self.v
        for c in range(2):
            S.op("dve", lambda: v.bn_stats(out=st[:, c, :], in_=z[:, c * 512:(c + 1) * 512]), reads=[z], writes=[st])
        S.op("dve", lambda: v.bn_aggr(out=mv[:], in_=st[:]), reads=[st], writes=[mv])
        S.op("dve", lambda: v.tensor_scalar(out=rstd[:], in0=mv[:, 1:2], scalar1=LN_EPS, scalar2=None, op0=ALU.add),
             reads=[mv], writes=[rstd])
        S.op("act", lambda: self.a.sqrt(out=rstd[:], in_=rstd[:]), reads=[rstd], writes=[rstd])

    def ln_b(self, z, out, gt, bt, mv, rstd):
        S, v = self.S, self.v
        S.op("dve", lambda: v.reciprocal(out=rstd[:], in_=rstd[:]), reads=[rstd], writes=[rstd])
        S.op("dve", lambda: v.tensor_scalar(out=out[:], in0=z[:], scalar1=mv[:, 0:1], scalar2=rstd[:, 0:1],
                                            op0=ALU.subtract, op1=ALU.mult), reads=[z, mv, rstd], writes=[out])
        S.op("dve", lambda: v.tensor_tensor(out=out[:], in0=out[:], in1=gt[:], op=ALU.mult), reads=[out, gt], writes=[out])
        S.op("dve", lambda: v.tensor_tensor(out=out[:], in0=out[:], in1=bt[:], op=ALU.add), reads=[out, bt], writes=[out])

    def layer_norm(self, z, out, gt, bt, st, mv, rstd, eng2="dve"):
        self.ln_a(z, st, mv, rstd)
        self.ln_b(z, out, gt, bt, mv, rstd)

    def router_alloc(self, l, T):
        S = self.S
        R = {}
        R["rw"] = S.sb("rw", [128, 8, E], F32)
        R["rb"] = S.sb("rb", [1, E], F32)
        S.dma("sp", R["rw"][:], T["router_w"].t.ap()[l].rearrange("(k p) e -> p k e", p=128), reads=[], writes=[R["rw"]])
        S.dma("sp", R["rb"][:], T["router_b"].t.ap()[l:l + 1, :], reads=[], writes=[R["rb"]])
        R["pTf"] = [S.ps(f"pTf{i}", [128, 4, 128], F32) for i in range(2)]
        R["plg"] = S.ps("plg", [128, E], F32)
        R["x1T"] = S.sb("x1T", [128, 8, 128], F32)
        R["lg"] = S.sb("lg", [128, E], F32)
        R["t8"] = S.sb("t8", [128, 8], F32)
        R["nmx"] = S.sb("nmx", [128, 1], F32)
        R["msk"] = S.sb("msk", [128, E], F32)
        R["ex"] = S.sb("ex", [128, E], F32)
        R["ssum"] = S.sb("ssum", [128, 1], F32)
        return R

    def router_tile(self, j, x1s, R):
        S, v = self.S, self.v
        for h in range(2):
            for k in range(4):
                kk = h * 4 + k
                S.op("pe", lambda: self.pe.transpose(out=R["pTf"][h][:, k, :], in_=x1s[:, kk * 128:(kk + 1) * 128],
                                                     identity=self.identf[:]), reads=[x1s, self.identf], writes=[R["pTf"][h]])
            S.op("act", lambda: self.a.copy(out=R["x1T"][:, h * 4:(h + 1) * 4, :], in_=R["pTf"][h][:]),
                 reads=[R["pTf"][h]], writes=[R["x1T"]])
        for k in range(8):
            S.op("pe", lambda: self.pe.matmul(R["plg"][:], lhsT=R["x1T"][:, k, :], rhs=R["rw"][:, k, :], start=(k == 0), stop=False),
                 reads=[R["x1T"], R["rw"]], writes=[R["plg"]])
        S.op("pe", lambda: self.pe.matmul(R["plg"][:], lhsT=self.onesf[0:1, :], rhs=R["rb"][0:1, :], start=False, stop=True),
             reads=[self.onesf, R["rb"]], writes=[R["plg"]])
        S.op("dve", lambda: v.tensor_copy(out=R["lg"][:], in_=R["plg"][:]), reads=[R["plg"]], writes=[R["lg"]])
        S.op("dve", lambda: v.max(out=R["t8"][:], in_=R["lg"][:]), reads=[R["lg"]], writes=[R["t8"]])
        S.op("dve", lambda: v.tensor_scalar(out=R["nmx"][:], in0=R["t8"][:, 0:1], scalar1=-1.0, scalar2=None, op0=ALU.mult),
             reads=[R["t8"]], writes=[R["nmx"]])
        S.op("dve", lambda: v.tensor_scalar(out=R["msk"][:], in0=R["lg"][:], scalar1=R["t8"][:, 3:4], scalar2=None, op0=ALU.is_ge),
             reads=[R["lg"], R["t8"]], writes=[R["msk"]])
        S.op("act", lambda: self.a.activation(out=R["ex"][:], in_=R["lg"][:], func=AF.Exp, bias=R["nmx"][:, 0:1], scale=1.0),
             reads=[R["lg"], R["nmx"]], writes=[R["ex"]])
        S.op("dve", lambda: v.tensor_tensor(out=R["ex"][:], in0=R["ex"][:], in1=R["msk"][:], op=ALU.mult),
             reads=[R["ex"], R["msk"]], writes=[R["ex"]])
        S.op("dve", lambda: v.reduce_sum(out=R["ssum"][:], in_=R["ex"][:], axis=AX.X), reads=[R["ex"]], writes=[R["ssum"]])
        S.op("dve", lambda: v.reciprocal(out=R["ssum"][:], in_=R["ssum"][:]), reads=[R["ssum"]], writes=[R["ssum"]])
        S.op("dve", lambda: v.tensor_scalar(out=self.gates_all[:, j, :], in0=R["ex"][:], scalar1=R["ssum"][:, 0:1], scalar2=None, op0=ALU.mult),
             reads=[R["ex"], R["ssum"]], writes=[self.gates_all])
        S.op("dve", lambda: v.tensor_copy(out=self.mask_all[:, j, :], in_=R["msk"][:]), reads=[R["msk"]], writes=[self.mask_all])

    def route_and_stage(self, j, x1s, R, xb, T):
        S = self.S
        S.op("act", lambda: self.a.copy(out=xb[:], in_=x1s[:]), reads=[x1s], writes=[xb])
        S.dma("sp", T["x1b"].t.ap()[j * 128:(j + 1) * 128, :], xb[:], reads=[xb])
        self.router_tile(j, x1s, R)

    def routing_tables(self, l):
        S, v, g = self.S, self.v, self.g
        with ExitStack() as es2:
            old = S.es
            S.es = es2
            rank = S.sb("rank", [128, NT, E], F32)
            tot = S.sb("tot", [128, NT, E], F32)
            pref = S.sb("pref", [128, NT, E], F32)
            val = S.sb("val", [128, NT, E], F32)
            cnt = S.sb("cnt", [128, E], F32)
            pad = S.sb("pad", [128, E], F32)
            pst = S.sb("pst", [128, E], F32)
            pend = S.sb("pend", [128, E], F32)
            t8a = S.sb("t8a", [128, NT, 8], F32)
            posk = S.sb("posk", [128, NT, 4], F32)
            sel = S.sb("sel", [128, NT, E], F32)
            blke = S.sb("blke", [128, NB], F32)
            sw = S.sb("sw", [128, NB], F32)
            base = S.sb("base", [128, NB], F32)
            pw = [S.ps(f"pw{i}", [128, 512], F32) for i in range(2)]
            pt = [S.ps(f"ptt{i}", [128, 512], F32) for i in range(2)]
            for c in range(4):
                rhs = self.mask_all[:, 16 * c:16 * c + 16, :].rearrange("p a b -> p (a b)")
                S.op("pe", lambda: self.pe.matmul(pw[c % 2][:], lhsT=self.lsb[:], rhs=rhs, start=True, stop=True),
                     reads=[self.lsb, self.mask_all], writes=[pw[c % 2]])
                S.op("pe", lambda: self.pe.matmul(pt[c % 2][:], lhsT=self.onesb[:], rhs=rhs, start=True, stop=True),
                     reads=[self.onesb, self.mask_all], writes=[pt[c % 2]])
                S.op("dve", lambda: v.tensor_copy(out=rank[:, 16 * c:16 * c + 16, :].rearrange("p a b -> p (a b)"), in_=pw[c % 2][:]),
                     reads=[pw[c % 2]], writes=[rank])
                S.op("act", lambda: self.a.copy(out=tot[:, 16 * c:16 * c + 16, :].rearrange("p a b -> p (a b)"), in_=pt[c % 2][:]),
                     reads=[pt[c % 2]], writes=[tot])
            S.op("dve", lambda: v.memset(pref[:, 0, :], 0.0), writes=[pref])
            for j in range(1, NT):
                S.op("dve", lambda: v.tensor_tensor(out=pref[:, j, :], in0=pref[:, j - 1, :], in1=tot[:, j - 1, :], op=ALU.add),
                     reads=[pref, tot], writes=[pref])
            S.op("dve", lambda: v.tensor_tensor(out=cnt[:], in0=pref[:, NT - 1, :], in1=tot[:, NT - 1, :], op=ALU.add),
                 reads=[pref, tot], writes=[cnt])
            cnti = S.sb("cnti", [128, E], I32)
            S.op("dve", lambda: v.tensor_copy(out=cnti[:], in_=cnt[:]), reads=[cnt], writes=[cnti])
            S.op("dve", lambda: v.tensor_scalar(out=cnti[:], in0=cnti[:], scalar1=BK - 1, scalar2=None, op0=ALU.add), reads=[cnti], writes=[cnti])
            S.op("dve", lambda: v.tensor_scalar(out=cnti[:], in0=cnti[:], scalar1=7, scalar2=7, op0=ALU.arith_shift_right, op1=ALU.logical_shift_left),
                 reads=[cnti], writes=[cnti])
            S.op("dve", lambda: v.tensor_copy(out=pad[:], in_=cnti[:]), reads=[cnti], writes=[pad])
            S.op("dve", lambda: v.memset(pst[:, 0:1], 0.0), writes=[pst])
            for e in range(1, E):
                S.op("dve", lambda: v.tensor_tensor(out=pst[:, e:e + 1], in0=pst[:, e - 1:e], in1=pad[:, e - 1:e], op=ALU.add),
                     reads=[pst, pad], writes=[pst])
            S.op("dve", lambda: v.tensor_tensor(out=pend[:], in0=pst[:], in1=pad[:], op=ALU.add), reads=[pst, pad], writes=[pend])
            S.op("dve", lambda: v.tensor_tensor(out=rank[:], in0=rank[:], in1=pref[:], op=ALU.add), reads=[rank, pref], writes=[rank])
            S.op("dve", lambda: v.tensor_tensor(out=rank[:], in0=rank[:], in1=pst[:, :].unsqueeze(1).to_broadcast([128, NT, E]), op=ALU.add),
                 reads=[rank, pst], writes=[rank])
            S.op("dve", lambda: v.tensor_scalar(out=val[:], in0=rank[:], scalar1=-1.0, scalar2=BIG, op0=ALU.mult, op1=ALU.add),
                 reads=[rank], writes=[val])
            S.op("dve", lambda: v.tensor_tensor(out=val[:], in0=val[:], in1=self.mask_all[:], op=ALU.mult),
                 reads=[val, self.mask_all], writes=[val])
            for j in range(NT):
                S.op("dve", lambda: v.max(out=t8a[:, j, :], in_=val[:, j, :]), reads=[val], writes=[t8a])
            S.op("dve", lambda: v.tensor_scalar(out=posk[:], in0=t8a[:, :, 0:4], scalar1=-1.0, scalar2=BIG, op0=ALU.mult, op1=ALU.add),
                 reads=[t8a], writes=[posk])
            S.op("dve", lambda: v.tensor_copy(out=self.idx_all[:], in_=posk[:]), reads=[posk], writes=[self.idx_all])
            for k in range(4):
                S.op("dve", lambda: v.tensor_tensor(out=sel[:], in0=val[:], in1=t8a[:, :, k:k + 1].to_broadcast([128, NT, E]), op=ALU.is_equal),
                     reads=[val, t8a], writes=[sel])
                S.op("dve", lambda: v.tensor_tensor(out=sel[:], in0=sel[:], in1=self.gates_all[:], op=ALU.mult),
                     reads=[sel, self.gates_all], writes=[sel])
                S.op("dve", lambda: v.tensor_reduce(out=self.gsel_all[:, :, k:k + 1], in_=sel[:], axis=AX.X, op=ALU.add),
                     reads=[sel], writes=[self.gsel_all])
            S.op("dve", lambda: v.memset(blke[:], 0.0), writes=[blke])
            for e in range(E):
                S.op("dve", lambda: v.scalar_tensor_tensor(out=blke[:], in0=self.bvals[:], scalar=pend[:, e:e + 1], in1=blke[:],
                                                           op0=ALU.is_ge, op1=ALU.add), reads=[self.bvals, pend, blke], writes=[blke])
            S.op("dve", lambda: v.tensor_scalar(out=blke[:], in0=blke[:], scalar1=float(E - 1), scalar2=None, op0=ALU.min),
                 reads=[blke], writes=[blke])
            S.op("dve", lambda: v.memset(sw[:, 0:1], 1.0), writes=[sw])
            S.op("dve", lambda: v.tensor_tensor(out=sw[:, 1:NB], in0=blke[:, 1:NB], in1=blke[:, 0:NB - 1], op=ALU.not_equal),
                 reads=[blke], writes=[sw])
            S.op("dve", lambda: v.tensor_scalar(out=sw[:], in0=sw[:], scalar1=-BIGI, scalar2=BIGI, op0=ALU.mult, op1=ALU.add),
                 reads=[sw], writes=[sw])
            S.op("dve", lambda: v.tensor_scalar(out=base[:], in0=blke[:], scalar1=128.0, scalar2=self.kp[:, 0:1], op0=ALU.mult, op1=ALU.add),
                 reads=[blke, self.kp], writes=[base])
            S.op("dve", lambda: v.tensor_tensor(out=base[:], in0=base[:], in1=sw[:], op=ALU.add), reads=[base, sw], writes=[base])
            S.op("dve", lambda: v.tensor_copy(out=self.widx[:], in_=base[:]), reads=[base], writes=[self.widx])
            S.op("dve", lambda: v.tensor_tensor(out=base[:], in0=blke[:], in1=sw[:], op=ALU.add), reads=[blke, sw], writes=[base])
            S.op("dve", lambda: v.tensor_copy(out=self.bidx[:], in_=base[:]), reads=[base], writes=[self.bidx])
            for nm, bf in (("gates_all", self.gates_all), ("idx_all", self.idx_all), ("gsel_all", self.gsel_all),
                           ("widx", self.widx), ("bidx", self.bidx), ("cnt", cnt), ("pst", pst), ("blke", blke)):
                if nm in self.dbg:
                    S.dma("sp", self.dbg[nm].t.ap(), bf[:], reads=[bf])
            S.barrier()
            S.es = old

    def dispatch(self, T):
        S = self.S
        with ExitStack() as es2:
            old = S.es
            S.es = es2
            xb = [S.sb(f"dxb{i}", [128, D], BF16) for i in range(3)]
            for j in range(NT):
                b = xb[j % 3]
                S.dma("sp", b[:], T["x1b"].t.ap()[j * 128:(j + 1) * 128, :], reads=[], writes=[b])
                for k in range(4):
                    S.dma("pool", T["xs"].t.ap(), b[:], reads=[b, self.idx_all], writes=[],
                          indirect=dict(**scatter(self.idx_all[:, j, k:k + 1]), bounds_check=self.reg_prow, oob_is_err=False))
            S.barrier()
            S.es = old

    def convert_weights(self, l, T):
        S = self.S
        for e in range(E):
            src = T["w1"].t.ap()[(l * E + e) * D:(l * E + e + 1) * D, :].rearrange("(k p) n -> p k n", p=128)
            dst = T["w1c"].t.ap()[e * 128:(e + 1) * 128, :].rearrange("p (k n) -> p k n", k=8)
            for hf in range(2):
                S.dma("bg", dst[:, hf * 4:(hf + 1) * 4, :], src[:, hf * 4:(hf + 1) * 4, :])
            src = T["w2"].t.ap()[(l * E + e) * D:(l * E + e + 1) * D, :].rearrange("(k p) n -> p k n", p=128)
            dst = T["w2c"].t.ap()[e * 128:(e + 1) * 128, :].rearrange("p (k n) -> p k n", k=8)
            for hf in range(2):
                S.dma("bg", dst[:, hf * 4:(hf + 1) * 4, :], src[:, hf * 4:(hf + 1) * 4, :])
        S.dma("bg", T["b1c"].t.ap(), T["b1"].t.ap()[l * E:(l + 1) * E, :])

    def experts(self, l, T):
        S, v, g, a, pe = self.S, self.v, self.g, self.a, self.pe
        with ExitStack() as es2:
            old = S.es
            S.es = es2
            S.wait_bg()
            W1 = S.sb("W1", [128, 8 * 2 * D], BF16)
            W2 = S.sb("W2", [128, 8 * D], BF16)
            B1 = S.sb("B1", [128, 2 * D], BF16)
            xsb = [S.sb(f"xsb{i}", [128, D], BF16) for i in range(3)]
            XT = [S.sb(f"XT{i}", [128, 8, 128], BF16) for i in range(2)]
            AT = [S.sb(f"AT{i}", [128, 8, 128], BF16) for i in range(2)]
            actb = [S.sb(f"actb{i}", [128, D], BF16) for i in range(2)]
            gl = [S.sb(f"gl{i}", [128, 256], F32) for i in range(2)]
            sg = [S.sb(f"sg{i}", [128, 256], F32) for i in range(2)]
            ln = [S.sb(f"ln{i}", [128, 256], F32) for i in range(2)]
            hb = [S.sb(f"hb{i}", [128, 512], F32) for i in range(2)]
            ysb = [S.sb(f"ysb{i}", [128, D], BF16) for i in range(2)]
            pT1 = S.ps("pT1", [128, 8, 128], BF16)
            pT2 = S.ps("pT2", [128, 8, 128], BF16)
            ph = [S.ps(f"ph{n}", [128, 512], F32) for n in range(4)]
            py = [S.ps(f"py{n}", [128, 512], F32) for n in range(2)]
            PRE = 2
            for b in range(min(PRE, NB)):
                S.dma("sp", xsb[b % 3][:], T["xs"].t.ap()[b * BK:(b + 1) * BK, :], reads=[], writes=[xsb[b % 3]])

            def t1(b):
                xs_ = xsb[b % 3]
                for k in range(8):
                    S.op("pe", lambda: pe.transpose(out=pT1[:, k, :], in_=xs_[:, k * 128:(k + 1) * 128], identity=self.ident[:]),
                         reads=[xs_, self.ident], writes=[pT1])
                S.op("act", lambda: a.copy(out=XT[b % 2][:], in_=pT1[:]), reads=[pT1], writes=[XT[b % 2]])

            def p1(b):
                xt = XT[b % 2]
                ab = actb[b % 2]
                for n in range(4):
                    for k in range(8):
                        S.op("pe", lambda: pe.matmul(ph[n][:], lhsT=xt[:, k, :], rhs=W1[:, k * 2048 + n * 512:k * 2048 + (n + 1) * 512], start=(k == 0), stop=False),
                             reads=[xt, W1], writes=[ph[n]])
                    S.op("pe", lambda: pe.matmul(ph[n][:], lhsT=self.inv128[:], rhs=B1[:, n * 512:(n + 1) * 512], start=False, stop=True),
                         reads=[self.inv128, B1], writes=[ph[n]])
                    i2 = n % 2
                    hv = ph[n][:].rearrange("p (c two) -> p c two", two=2)
                    S.op("dve", lambda: v.tensor_scalar(out=gl[i2][:], in0=hv[:, :, 0], scalar1=7.0, scalar2=None, op0=ALU.min),
                         reads=[ph[n]], writes=[gl[i2]])
                    S.op("dve", lambda: v.tensor_scalar(out=ln[i2][:], in0=hv[:, :, 1], scalar1=-7.0, scalar2=7.0, op0=ALU.max, op1=ALU.min),
                         reads=[ph[n]], writes=[ln[i2]])
                    S.op("act", lambda: a.activation(out=sg[i2][:], in_=gl[i2][:], func=AF.Sigmoid, scale=1.702),
                         reads=[gl[i2]], writes=[sg[i2]])
                    S.op("act", lambda: a.activation(out=ln[i2][:], in_=ln[i2][:], func=AF.Identity, bias=1.0, scale=1.0),
                         reads=[ln[i2]], writes=[ln[i2]])
                    S.op("dve", lambda: v.tensor_tensor(out=sg[i2][:], in0=sg[i2][:], in1=gl[i2][:], op=ALU.mult),
                         reads=[sg[i2], gl[i2]], writes=[sg[i2]])
                    S.op("dve", lambda: v.tensor_tensor(out=ab[:, n * 256:(n + 1) * 256], in0=ln[i2][:], in1=sg[i2][:], op=ALU.mult),
                         reads=[ln[i2], sg[i2]], writes=[ab])

            def t2(b):
                ab = actb[b % 2]
                for k in range(8):
                    S.op("pe", lambda: pe.transpose(out=pT2[:, k, :], in_=ab[:, k * 128:(k + 1) * 128], identity=self.ident[:]),
                         reads=[ab, self.ident], writes=[pT2])
                S.op("act", lambda: a.copy(out=AT[b % 2][:], in_=pT2[:]), reads=[pT2], writes=[AT[b % 2]])

            def p2(b):
                at = AT[b % 2]
                y = ysb[b % 2]
                for n in range(2):
                    for k in range(8):
                        S.op("pe", lambda: pe.matmul(py[n][:], lhsT=at[:, k, :], rhs=W2[:, k * 1024 + n * 512:k * 1024 + (n + 1) * 512], start=(k == 0), stop=(k == 7)),
                             reads=[at, W2], writes=[py[n]])
                    if n == 0:
                        S.op("dve", lambda: v.tensor_copy(out=y[:, 0:512], in_=py[0][:]), reads=[py[0]], writes=[y])
                    else:
                        S.op("act", lambda: a.copy(out=y[:, 512:1024], in_=py[1][:]), reads=[py[1]], writes=[y])
                S.dma("sp", T["ys"].t.ap()[b * BK:(b + 1) * BK, :], y[:], reads=[y], writes=[])

            def g1(b):
                S.dma("pool", W1[:], T["w1c"].t.ap(), reads=[self.widx], writes=[W1],
                      indirect=dict(**gather(self.widx[:, b:b + 1]), bounds_check=self.reg_wrow, oob_is_err=False))
                S.dma("pool", B1[:], T["b1c"].t.ap(), reads=[self.bidx], writes=[B1],
                      indirect=dict(**gather(self.bidx[:, b:b + 1]), bounds_check=self.reg_brow, oob_is_err=False))

            def g2(b):
                S.dma("pool", W2[:], T["w2c"].t.ap(), reads=[self.widx], writes=[W2],
                      indirect=dict(**gather(self.widx[:, b:b + 1]), bounds_check=self.reg_wrow, oob_is_err=False))

            g1(0)
            t1(0)
            for b in range(NB):
                if b + PRE < NB:
                    bb = b + PRE
                    S.dma("sp", xsb[bb % 3][:], T["xs"].t.ap()[bb * BK:(bb + 1) * BK, :], reads=[], writes=[xsb[bb % 3]])
                p1(b)
                if b + 1 < NB:
                    g1(b + 1)
                if b >= 1:
                    p2(b - 1)
                g2(b)
                if b + 1 < NB:
                    t1(b + 1)
                t2(b)
            p2(NB - 1)
            S.barrier()
            S.es = old

    def combine(self, l, T, xout):
        S, v, g = self.S, self.v, self.g
        with ExitStack() as es2:
            old = S.es
            S.es = es2
            gt = S.sb("c_g", [128, D], F32)
            bt = S.sb("c_b", [128, D], F32)
            S.dma("sp", gt[:], T["ln_g"].t.ap()[l, 1:2, :].partition_broadcast(128), reads=[], writes=[gt])
            S.dma("sp", bt[:], T["ln_b"].t.ap()[l, 1:2, :].partition_broadcast(128), reads=[], writes=[bt])
            NBUF = 2
            yk = [[S.sb(f"yk{i}_{k}", [128, D], BF16) for k in range(4)] for i in range(NBUF)]
            dg = [[S.sb(f"dg{i}_{k}", [128, 128], BF16) for k in range(4)] for i in range(NBUF)]
            x1 = [S.sb(f"cx1_{i}", [128, D], F32) for i in range(NBUF)]
            acc = [S.sb(f"acc{i}", [128, D], F32) for i in range(NBUF)]
            xo = [S.sb(f"cxo{i}", [128, D], F32) for i in range(NBUF)]
            st = [S.sb(f"c_st{i}", [128, 2, 6], F32) for i in range(2)]
            mv = [S.sb(f"c_mv{i}", [128, 2], F32) for i in range(2)]
            rstd = [S.sb(f"c_rstd{i}", [128, 1], F32) for i in range(2)]
            b2f = S.sb("c_b2f", [E, D], F32)
            S.dma("sp", b2f[:], T["b2"].t.ap()[l * E:(l + 1) * E, :], writes=[b2f])
            b2s = S.sb("c_b2s", [E, D], BF16)
            S.op("dve", lambda: v.tensor_copy(out=b2s[:], in_=b2f[:]), reads=[b2f], writes=[b2s])
            pgT = S.ps("c_pgT", [E, 128], F32)
            gT = [S.sb(f"c_gT{i}", [E, 128], BF16) for i in range(2)]
            pb = [[S.ps(f"c_pb{i}_{n}", [128, 512], F32) for n in range(2)] for i in range(2)]

            def loads(j):
                i = j % NBUF
                S.dma("sp", x1[i][:], T["x1"].t.ap()[j * 128:(j + 1) * 128, :], reads=[], writes=[x1[i]])
                for k in range(4):
                    S.dma("pool", yk[i][k][:], T["ys"].t.ap(), reads=[self.idx_all], writes=[yk[i][k]],
                          indirect=dict(**gather(self.idx_all[:, j, k:k + 1]), bounds_check=self.reg_prow, oob_is_err=False))
            loads(0)
            for j in range(NT):
                i = j % NBUF
                if j + 1 < NT:
                    loads(j + 1)
                S.op("pe", lambda: self.pe.transpose(out=pgT[:], in_=self.gates_all[:, j, :], identity=self.identf[:]),
                     reads=[self.gates_all, self.identf], writes=[pgT])
                S.op("act", lambda: self.a.copy(out=gT[i][:], in_=pgT[:]), reads=[pgT], writes=[gT[i]])
                for k in range(4):
                    S.op("act", lambda: self.a.activation(out=dg[i][k][:], in_=self.ident[:], func=AF.Copy, scale=self.gsel_all[:, j, k:k + 1]),
                         reads=[self.ident, self.gsel_all], writes=[dg[i][k]])
                for n in range(2):
                    S.op("pe", lambda: self.pe.matmul(pb[i][n][:], lhsT=gT[i][:], rhs=b2s[:, n * 512:(n + 1) * 512], start=True, stop=False),
                         reads=[gT[i], b2s], writes=[pb[i][n]])
                    for k in range(4):
                        S.op("pe", lambda: self.pe.matmul(pb[i][n][:], lhsT=dg[i][k][:], rhs=yk[i][k][:, n * 512:(n + 1) * 512], start=False, stop=(k == 3)),
                             reads=[dg[i][k], yk[i][k]], writes=[pb[i][n]])
                    S.op("dve", lambda: v.scalar_tensor_tensor(out=acc[i][:, n * 512:(n + 1) * 512], in0=x1[i][:, n * 512:(n + 1) * 512], scalar=ALPHA,
                                                               in1=pb[i][n][:], op0=ALU.mult, op1=ALU.add),
                         reads=[x1[i], pb[i][n]], writes=[acc[i]])
                self.ln_a(acc[i], st[i], mv[i], rstd[i])
                if j >= 1:
                    ip = (j - 1) % NBUF
                    self.ln_b(acc[ip], xo[ip], gt, bt, mv[ip], rstd[ip])
                    S.dma("sp", xout.t.ap()[(j - 1) * 128:j * 128, :], xo[ip][:], reads=[xo[ip]], writes=[])
            ip = (NT - 1) % NBUF
            self.ln_b(acc[ip], xo[ip], gt, bt, mv[ip], rstd[ip])
            S.dma("sp", xout.t.ap()[(NT - 1) * 128:NT * 128, :], xo[ip][:], reads=[xo[ip]], writes=[])
            S.barrier()
            S.es = old

    def router_pass(self, l, T):
        S, v, g = self.S, self.v, self.g
        with ExitStack() as es2:
            old = S.es
            S.es = es2
            R = self.router_alloc(l, T)
            xs_ = [S.sb(f"rx{i}", [128, D], F32) for i in range(3)]
            xb = [S.sb(f"rxb{i}", [128, D], BF16) for i in range(2)]
            PRE = 2
            for j in range(min(PRE, NT)):
                S.dma("sp", xs_[j % 3][:], T["x1"].t.ap()[j * 128:(j + 1) * 128, :], writes=[xs_[j % 3]])
            for j in range(NT):
                if j + PRE < NT:
                    jj = j + PRE
                    S.dma("sp", xs_[jj % 3][:], T["x1"].t.ap()[jj * 128:(jj + 1) * 128, :], writes=[xs_[jj % 3]])
                x = xs_[j % 3]
                b = xb[j % 2]
                S.op("act", lambda: self.a.copy(out=b[:], in_=x[:]), reads=[x], writes=[b])
                S.dma("sp", T["x1b"].t.ap()[j * 128:(j + 1) * 128, :], b[:], reads=[b])
                self.router_tile(j, x, R)
            S.barrier()
            S.es = old

    def moe_layer(self, l, T, xout):
        if not self.fuse_router:
            self.router_pass(l, T)
        self.routing_tables(l)
        self.dispatch(T)
        self.experts(l, T)
        self.combine(l, T, xout)

    def load_w_bf16(self, dst, src_ap, ncols, col0=0, q="pool"):
        S = self.S
        v = src_ap.rearrange("(k p) n -> p k n", p=128)
        step = 512
        for c0 in range(0, ncols, step):
            for kh in range(2):
                S.dma(q, dst[:, kh * 4:(kh + 1) * 4, c0:c0 + step], v[:, kh * 4:(kh + 1) * 4, col0 + c0:col0 + c0 + step], writes=[dst])

    def xT_chunk(self, xin, t0, xs_, xb_, pT, xT, ntile=4):
        S, v, g, a, pe = self.S, self.v, self.g, self.a, self.pe
        for t in range(ntile):
            xs = xs_[t % len(xs_)]
            xb = xb_[t % len(xb_)]
            S.dma("sp", xs[:], xin.t.ap()[t0 + t * 128:t0 + (t + 1) * 128, :], writes=[xs])
            S.op("dve", lambda: v.tensor_copy(out=xb[:], in_=xs[:]), reads=[xs], writes=[xb])
            p = pT[t % len(pT)]
            for k in range(8):
                S.op("pe", lambda: pe.transpose(out=p[:, k, :], in_=xb[:, k * 128:(k + 1) * 128], identity=self.ident[:]),
                     reads=[xb, self.ident], writes=[p])
            S.op("act", lambda: a.copy(out=xT[:, :, t * 128:(t + 1) * 128], in_=p[:]), reads=[p], writes=[xT])

    def conv_layer(self, jl, l, T, xin, xout, hook=None):
        S, v, g, a, pe = self.S, self.v, self.g, self.a, self.pe
        NCH = NTOK // 512
        with ExitStack() as es2:
            old = S.es
            S.es = es2
            win = S.sb("cv_win", [128, 8, 3 * D], BF16)
            self.load_w_bf16(win, T["conv_w_in"].t.ap()[jl], 3 * D)
            if hook is not None:
                hook()
            xs_ = [S.sb(f"cv_xs{i}", [128, D], F32) for i in range(2)]
            xb_ = [S.sb(f"cv_xb{i}", [128, D], BF16) for i in range(2)]
            pT = [S.ps(f"cv_pT{i}", [128, 8, 128], BF16) for i in range(2)]
            xT = [S.sb(f"cv_xT{i}", [128, 8, 512], BF16) for i in range(2)]
            ph = [[S.ps(f"cv_ph{i}_{p}", [128, 512], F32) for p in range(3)] for i in range(2)]
            gbs = [S.sb(f"cv_gbs{i}", [128, 512], F32) for i in range(2)]
            us = [S.sb(f"cv_us{i}", [128, 512], F32) for i in range(2)]
            zs = [S.sb(f"cv_zs{i}", [128, 512], F32) for i in range(2)]
            self.xT_chunk(xin, 0, xs_, xb_, pT, xT[0])
            for ch in range(NCH):
                xt = xT[ch % 2]
                if ch + 1 < NCH:
                    self.xT_chunk(xin, (ch + 1) * 512, xs_, xb_, pT, xT[(ch + 1) % 2])
                for c in range(8):
                    i = c % 2
                    for p in range(3):
                        f0 = p * D + c * 128
                        for k in range(8):
                            S.op("pe", lambda: pe.matmul(ph[i][p][:], lhsT=win[:, k, f0:f0 + 128], rhs=xt[:, k, :], start=(k == 0), stop=(k == 7)),
                                 reads=[win, xt], writes=[ph[i][p]])
                    S.op("act", lambda: a.copy(out=gbs[i][:], in_=ph[i][0][:]), reads=[ph[i][0]], writes=[gbs[i]])
                    S.op("act", lambda: a.copy(out=us[i][:], in_=ph[i][2][:]), reads=[ph[i][2]], writes=[us[i]])
                    S.op("dve", lambda: v.tensor_tensor(out=zs[i][:], in0=ph[i][1][:], in1=us[i][:], op=ALU.mult),
                         reads=[ph[i][1], us[i]], writes=[zs[i]])
                    S.dma("sp", T["cv_gb"].t.ap()[c * 128:(c + 1) * 128, ch * 512:(ch + 1) * 512], gbs[i][:], reads=[gbs[i]])
                    S.dma("sp", T["cv_z"].t.ap()[c * 128:(c + 1) * 128, ch * 512:(ch + 1) * 512], zs[i][:], reads=[zs[i]])
            S.barrier()
            S.es = old
        with ExitStack() as es2:
            old = S.es
            S.es = es2
            wout = S.sb("cv_wout", [128, 8, D], BF16)
            self.load_w_bf16(wout, T["conv_w_out"].t.ap()[jl], D)
            cw = S.sb("cv_cw", [128, 8, 3], F32)
            with self.nc.allow_non_contiguous_dma(reason="tiny conv weight transpose load"):
                for j3 in range(3):
                    S.dma("sp", cw[:, :, j3], T["conv_w"].t.ap()[jl, j3].rearrange("(c p) -> p c", p=128), writes=[cw])
            gt = S.sb("cv_g", [128, D], F32)
            bt = S.sb("cv_b", [128, D], F32)
            S.dma("sp", gt[:], T["ln_g"].t.ap()[l, 0:1, :].partition_broadcast(128), writes=[gt])
            S.dma("sp", bt[:], T["ln_b"].t.ap()[l, 0:1, :].partition_broadcast(128), writes=[bt])
            zt = [S.sb(f"cv_zt{i}", [128, 8, 514], F32) for i in range(2)]
            gbt = [S.sb(f"cv_gbt{i}", [128, 8, 512], F32) for i in range(2)]
            zc = [S.sb(f"cv_zc{i}", [128, 512], F32) for i in range(2)]
            gT = [S.sb(f"cv_gT{i}", [128, 8, 512], BF16) for i in range(2)]
            xr = [S.sb(f"cv_xr{i}", [128, D], F32) for i in range(2)]
            z1 = [S.sb(f"cv_z1{i}", [128, D], F32) for i in range(2)]
            xo = [S.sb(f"cv_xo{i}", [128, D], F32) for i in range(2)]
            py = [[S.ps(f"cv_py{i}_{n}", [128, 512], F32) for n in range(2)] for i in range(2)]
            st = [S.sb(f"cv_st{i}", [128, 2, 6], F32) for i in range(2)]
            mv = [S.sb(f"cv_mv{i}", [128, 2], F32) for i in range(2)]
            rstd = [S.sb(f"cv_rstd{i}", [128, 1], F32) for i in range(2)]
            pend = []
            R = self.router_alloc(l, T) if self.fuse_router else None
            rxb = [S.sb(f"cv_rxb{i}", [128, D], BF16) for i in range(2)]
            CPB = SEQ // 512
            for ch in range(NCH):
                i = ch % 2
                z_, gb_ = zt[i], gbt[i]
                t0 = ch * 512
                first = (ch % CPB == 0)
                last = (ch % CPB == CPB - 1)
                lo = 1 if first else 0
                hi = 513 if last else 514
                if first:
                    S.op("dve", lambda: v.memset(z_[:, :, 0:1], 0.0), writes=[z_])
                if last:
                    S.op("dve", lambda: v.memset(z_[:, :, 513:514], 0.0), writes=[z_])
                S.dma("sp", z_[:, :, lo:hi], T["cv_z"].t.ap()[:, t0 - 1 + lo:t0 - 1 + hi].rearrange("(c p) t -> p c t", p=128), writes=[z_])
                S.dma("sp", gb_[:], T["cv_gb"].t.ap()[:, t0:t0 + 512].rearrange("(c p) t -> p c t", p=128), writes=[gb_])
                g_ = gT[i]
                for c in range(8):
                    zz = zc[c % 2]
                    S.op("act", lambda: a.activation(out=zz[:], in_=z_[:, c, 0:512], func=AF.Copy, scale=cw[:, c, 0:1]),
                         reads=[z_, cw], writes=[zz])
                    S.op("dve", lambda: v.scalar_tensor_tensor(out=zz[:], in0=z_[:, c, 1:513], scalar=cw[:, c, 1:2], in1=zz[:], op0=ALU.mult, op1=ALU.add),
                         reads=[z_, cw, zz], writes=[zz])
                    S.op("dve", lambda: v.scalar_tensor_tensor(out=zz[:], in0=z_[:, c, 2:514], scalar=cw[:, c, 2:3], in1=zz[:], op0=ALU.mult, op1=ALU.add),
                         reads=[z_, cw, zz], writes=[zz])
                    S.op("dve", lambda: v.tensor_tensor(out=g_[:, c, :], in0=zz[:], in1=gb_[:, c, :], op=ALU.mult),
                         reads=[zz, gb_], writes=[g_])
                for t in range(4):
                    ti = (ch * 4 + t) % 2
                    tok0 = t0 + t * 128
                    S.dma("sp", xr[ti][:], xin.t.ap()[tok0:tok0 + 128, :], writes=[xr[ti]])
                    for n in range(2):
                        for c in range(8):
                            S.op("pe", lambda: pe.matmul(py[ti][n][:], lhsT=g_[:, c, t * 128:(t + 1) * 128], rhs=wout[:, c, n * 512:(n + 1) * 512],
                                                         start=(c == 0), stop=(c == 7)), reads=[g_, wout], writes=[py[ti][n]])
                        S.op("dve", lambda: v.scalar_tensor_tensor(out=z1[ti][:, n * 512:(n + 1) * 512], in0=xr[ti][:, n * 512:(n + 1) * 512], scalar=ALPHA,
                                                                   in1=py[ti][n][:], op0=ALU.mult, op1=ALU.add),
                             reads=[xr[ti], py[ti][n]], writes=[z1[ti]])
                    self.ln_a(z1[ti], st[ti], mv[ti], rstd[ti])

                    def fin(ti=ti, tok0=tok0, jt=ch * 4 + t):
                        self.ln_b(z1[ti], xo[ti], gt, bt, mv[ti], rstd[ti])
                        S.dma("sp", xout.t.ap()[tok0:tok0 + 128, :], xo[ti][:], reads=[xo[ti]])
                        if R is not None:
                            self.route_and_stage(jt, xo[ti], R, rxb[ti], T)
                    if pend:
                        pend.pop()()
                    pend.append(fin)
            pend.pop()()
            S.barrier()
            S.es = old

    def attn_layer(self, jl, l, T, xin, xout, hook=None):
        S, v, g, a, pe = self.S, self.v, self.g, self.a, self.pe
        NCH = NTOK // 512
        lambda_init = 0.8 - 0.6 * math.exp(-0.3 * l)
        with ExitStack() as es2:
            old = S.es
            S.es = es2
            win = S.sb("at_win", [128, 8, 3 * D], BF16)
            self.load_w_bf16(win, T["attn_w_in"].t.ap()[jl], 3 * D)
            if hook is not None:
                hook()
            xs_ = [S.sb(f"at_xs{i}", [128, D], F32) for i in range(2)]
            xb_ = [S.sb(f"at_xb{i}", [128, D], BF16) for i in range(2)]
            pT = [S.ps(f"at_pT{i}", [128, 8, 128], BF16) for i in range(2)]
            xT = [S.sb(f"at_xT{i}", [128, 8, 512], BF16) for i in range(2)]
            ph = [S.ps(f"at_ph{i}", [128, 512], F32) for i in range(4)]
            qs_ = [S.sb(f"at_qs{i}", [128, 512], BF16) for i in range(4)]
            vs_ = [S.sb(f"at_vs{i}", [128, D], BF16) for i in range(2)]
            cnt = 0
            self.xT_chunk(xin, 0, xs_, xb_, pT, xT[0])
            for ch in range(NCH):
                xt = xT[ch % 2]
                if ch + 1 < NCH:
                    self.xT_chunk(xin, (ch + 1) * 512, xs_, xb_, pT, xT[(ch + 1) % 2])
                for f in range(16):
                    i = cnt % 4
                    cnt += 1
                    for k in range(8):
                        S.op("pe", lambda: pe.matmul(ph[i][:], lhsT=win[:, k, f * 128:(f + 1) * 128], rhs=xt[:, k, :], start=(k == 0), stop=(k == 7)),
                             reads=[win, xt], writes=[ph[i]])
                    if f % 2 == 0:
                        S.op("act", lambda: a.copy(out=qs_[i][:], in_=ph[i][:]), reads=[ph[i]], writes=[qs_[i]])
                    else:
                        S.op("dve", lambda: v.tensor_copy(out=qs_[i][:], in_=ph[i][:]), reads=[ph[i]], writes=[qs_[i]])
                    S.dma("sp", T["at_qk"].t.ap()[f, :, ch * 512:(ch + 1) * 512], qs_[i][:], reads=[qs_[i]])
                for t in range(4):
                    vv = vs_[t % 2]
                    for n in range(2):
                        i = cnt % 4
                        cnt += 1
                        for k in range(8):
                            S.op("pe", lambda: pe.matmul(ph[i][:], lhsT=xt[:, k, t * 128:(t + 1) * 128], rhs=win[:, k, 2 * D + n * 512:2 * D + (n + 1) * 512],
                                                         start=(k == 0), stop=(k == 7)), reads=[win, xt], writes=[ph[i]])
                        if n == 0:
                            S.op("act", lambda: a.copy(out=vv[:, 0:512], in_=ph[i][:]), reads=[ph[i]], writes=[vv])
                        else:
                            S.op("dve", lambda: v.tensor_copy(out=vv[:, 512:1024], in_=ph[i][:]), reads=[ph[i]], writes=[vv])
                    tok0 = ch * 512 + t * 128
                    S.dma("sp", T["at_v"].t.ap()[tok0:tok0 + 128, :], vv[:], reads=[vv])
            S.barrier()
            S.es = old
        with ExitStack() as es2:
            old = S.es
            S.es = es2
            lamt = S.sb("at_lamt", [128, 256], F32)
            S.dma("sp", lamt[:], T["attn_lambda"].t.ap()[jl:jl + 1].rearrange("o a b -> o (a b)").partition_broadcast(128), writes=[lamt])
            lprod = S.sb("at_lprod", [128, 2, 64], F32)
            lsum = S.sb("at_lsum", [128, 2], F32)
            lv4 = lamt[:].rearrange("p (a b) -> p a b", a=4)
            S.op("dve", lambda: v.tensor_tensor(out=lprod[:, 0, :], in0=lv4[:, 0, :], in1=lv4[:, 1, :], op=ALU.mult), reads=[lamt], writes=[lprod])
            S.op("dve", lambda: v.tensor_tensor(out=lprod[:, 1, :], in0=lv4[:, 2, :], in1=lv4[:, 3, :], op=ALU.mult), reads=[lamt, lprod], writes=[lprod])
            S.op("dve", lambda: v.tensor_reduce(out=lsum[:], in_=lprod[:], axis=AX.X, op=ALU.add), reads=[lprod], writes=[lsum])
            S.op("act", lambda: a.activation(out=lsum[:], in_=lsum[:], func=AF.Exp), reads=[lsum], writes=[lsum])
            nlam = S.sb("at_nlam", [128, 1], F32)
            S.op("dve", lambda: v.tensor_tensor(out=nlam[:], in0=lsum[:, 1:2], in1=lsum[:, 0:1], op=ALU.subtract), reads=[lsum], writes=[nlam])
            S.op("dve", lambda: v.tensor_scalar(out=nlam[:], in0=nlam[:], scalar1=-lambda_init, scalar2=None, op0=ALU.add), reads=[nlam], writes=[nlam])
            gsub = S.sb("at_gsub", [128, 128], F32)
            S.dma("sp", gsub[:], T["attn_subln"].t.ap()[jl:jl + 1, :].partition_broadcast(128), writes=[gsub])
            S.op("dve", lambda: v.tensor_scalar(out=gsub[:], in0=gsub[:], scalar1=1.0 - lambda_init, scalar2=None, op0=ALU.mult), reads=[gsub], writes=[gsub])
            cb = S.sb("at_cb", [128, 16], F32)
            S.dma("sp", cb[:], T["cbias"].t.ap(), writes=[cb])
            zerob = S.sb("at_zero", [128, 512], BF16)
            S.op("dve", lambda: v.memset(zerob[:], 0.0), writes=[zerob])
            qT = [[S.sb(f"at_qT{i}_{m}", [128, SEQ], BF16) for m in range(2)] for i in range(2)]
            for i in range(2):
                S.op("dve", lambda: v.memset(qT[i][0][64:128, :], 0.0), writes=[qT[i][0]])
                S.op("dve", lambda: v.memset(qT[i][1][0:64, :], 0.0), writes=[qT[i][1]])
            kT = [S.sb(f"at_kT{i}", [128, SEQ], BF16) for i in range(2)]
            va = [S.sb(f"at_va{i}", [128, 32, 129], BF16) for i in range(2)]
            for i in range(2):
                S.op("dve", lambda: v.memset(va[i][:, :, 128:129], 1.0), writes=[va[i]])
            Tf = [S.sb(f"at_Tf{i}", [128, 1152], F32) for i in range(2)]
            Thi = [S.sb(f"at_Thi{i}", [128, 1152], BF16) for i in range(2)]
            Tlo = [S.sb(f"at_Tlo{i}", [128, 1152], BF16) for i in range(2)]
            Sps = [[S.ps(f"at_S{m}_{i}", [128, 512], F32) for i in range(2)] for m in range(2)]
            Ob = [S.ps(f"at_O{i}", [128, 512], F32) for i in range(3)]
            PT = [[S.sb(f"at_PT{m}_{i}", [128, 512], BF16) for i in range(3)] for m in range(2)]
            Osb = [S.sb(f"at_Osb{i}", [128, 8, 129], F32) for i in range(2)]
            rr = S.sb("at_rr", [128, 8], F32)
            o_ = [S.sb(f"at_o{i}", [128, 128], F32) for i in range(4)]
            sq = S.sb("at_sq", [128, 128], F32)
            ss = S.sb("at_ss", [128, 4], F32)
            aob = [S.sb(f"at_aob{i}", [128, 4, 128], BF16) for i in range(2)]

            def load_bh(ih):
                b, h = divmod(ih, 8)
                i = ih % 2
                S.dma("sp", qT[i][0][0:64, :], T["at_qk"].t.ap()[h, 0:64, b * SEQ:(b + 1) * SEQ], writes=[qT[i][0]])
                S.dma("sp", qT[i][1][64:128, :], T["at_qk"].t.ap()[h, 64:128, b * SEQ:(b + 1) * SEQ], writes=[qT[i][1]])
                S.dma("sp", kT[i][:], T["at_qk"].t.ap()[8 + h, :, b * SEQ:(b + 1) * SEQ], writes=[kT[i]])
                S.dma("sp", va[i][:, :, 0:128], T["at_v"].t.ap()[b * SEQ:(b + 1) * SEQ, h * 128:(h + 1) * 128].rearrange("(i p) c -> p i c", p=128),
                      writes=[va[i]])
                S.dma("sp", Tf[i][:], T["biasT"].t.ap()[h], writes=[Tf[i]])
                S.op("dve", lambda: v.tensor_scalar(out=Tf[i][:], in0=Tf[i][:], scalar1=8.0, scalar2=None, op0=ALU.mult), reads=[Tf[i]], writes=[Tf[i]])
                S.op("dve", lambda: v.tensor_copy(out=Thi[i][:], in_=Tf[i][:]), reads=[Tf[i]], writes=[Thi[i]])
                S.op("dve", lambda: v.tensor_tensor(out=Tf[i][:], in0=Tf[i][:], in1=Thi[i][:], op=ALU.subtract), reads=[Tf[i], Thi[i]], writes=[Tf[i]])
                S.op("dve", lambda: v.tensor_copy(out=Tlo[i][:], in_=Tf[i][:]), reads=[Tf[i]], writes=[Tlo[i]])

            NBH = 2 * 8
            load_bh(0)
            step = 0
            for ih in range(NBH):
                b, h = divmod(ih, 8)
                i = ih % 2
                if ih + 1 < NBH:
                    load_bh(ih + 1)
                q_, k_, v_, thi, tlo = qT[i], kT[i], va[i], Thi[i], Tlo[i]
                for j in range(8):
                    def qk(ki):
                        d = ki - 4 * j
                        near = (-1 <= d <= 4)
                        for m in range(2):
                            sp_ = Sps[m][ki % 2]
                            S.op("pe", lambda: pe.matmul(sp_[:], lhsT=k_[:, ki * 128:(ki + 1) * 128],
                                                         rhs=q_[m][:, j * 512:(j + 1) * 512], start=True, stop=not near),
                                 reads=[k_, q_[m]], writes=[sp_])
                        for m in range(2):
                            sp_ = Sps[m][ki % 2]
                            pt_ = PT[m][ki % 3]
                            if near:
                                cs = 512 - 128 * d
                                S.op("pe", lambda: pe.matmul(sp_[:], lhsT=self.ident[:], rhs=thi[:, cs:cs + 512], start=False, stop=True),
                                     reads=[self.ident, thi], writes=[sp_])
                                S.op("act", lambda: a.activation(out=pt_[:], in_=sp_[:], func=AF.Exp, scale=0.125), reads=[sp_], writes=[pt_])
                            else:
                                col = 2 * h + (1 if d > 0 else 0)
                                S.op("act", lambda: a.activation(out=pt_[:], in_=sp_[:], func=AF.Exp, bias=cb[:, col:col + 1], scale=0.125),
                                     reads=[sp_, cb], writes=[pt_])

                    def av(ki):
                        for m in range(2):
                            pt_ = PT[m][ki % 3]
                            for qs in range(4):
                                acc = m * 4 + qs
                                ob = Ob[acc // 3]
                                c0 = (acc % 3) * 129
                                S.op("pe", lambda: pe.matmul(ob[:, c0:c0 + 129], lhsT=pt_[:, qs * 128:(qs + 1) * 128], rhs=v_[:, ki, :],
                                                             start=False, stop=(ki == 31), skip_group_check=True),
                                     reads=[pt_, v_], writes=[ob])
                    qk(0)
                    for ob in Ob:
                        S.op("pe", lambda: pe.matmul(ob[:], lhsT=zerob[:, 0:128], rhs=zerob[:], start=True, stop=True), reads=[zerob], writes=[ob])
                    for ki in range(32):
                        if ki + 1 < 32:
                            qk(ki + 1)
                        av(ki)
                    os_ = Osb[step % 2]
                    ab_ = aob[step % 2]
                    step += 1
                    for bi in range(3):
                        na = 3 if bi < 2 else 2
                        eng, en = ("dve", v.tensor_copy) if bi != 1 else ("act", a.copy)
                        S.op(eng, lambda: en(out=os_[:, bi * 3:bi * 3 + na, :], in_=Ob[bi][:, 0:na * 129].rearrange("p (a c) -> p a c", c=129)),
                             reads=[Ob[bi]], writes=[os_])
                    S.op("dve", lambda: v.reciprocal(out=rr[:], in_=os_[:, :, 128]), reads=[os_], writes=[rr])
                    S.op("dve", lambda: v.tensor_scalar(out=rr[:, 4:8], in0=rr[:, 4:8], scalar1=nlam[:, 0:1], scalar2=None, op0=ALU.mult),
                         reads=[rr, nlam], writes=[rr])
                    for qs in range(4):
                        S.op("dve", lambda: v.tensor_scalar(out=o_[qs][:], in0=os_[:, qs, 0:128], scalar1=rr[:, qs:qs + 1], scalar2=None, op0=ALU.mult),
                             reads=[os_, rr], writes=[o_[qs]])
                        S.op("dve", lambda: v.scalar_tensor_tensor(out=o_[qs][:], in0=os_[:, 4 + qs, 0:128], scalar=rr[:, 4 + qs:5 + qs], in1=o_[qs][:],
                                                                   op0=ALU.mult, op1=ALU.add), reads=[os_, rr, o_[qs]], writes=[o_[qs]])
                        S.op("dve", lambda: v.tensor_tensor(out=sq[:], in0=o_[qs][:], in1=o_[qs][:], op=ALU.mult), reads=[o_[qs]], writes=[sq])
                        S.op("dve", lambda: v.reduce_sum(out=ss[:, qs:qs + 1], in_=sq[:], axis=AX.X), reads=[sq], writes=[ss])
                    S.op("dve", lambda: v.tensor_scalar(out=ss[:], in0=ss[:], scalar1=1.0 / 128.0, scalar2=LN_EPS, op0=ALU.mult, op1=ALU.add),
                         reads=[ss], writes=[ss])
                    S.op("act", lambda: a.sqrt(out=ss[:], in_=ss[:]), reads=[ss], writes=[ss])
                    S.op("dve", lambda: v.reciprocal(out=ss[:], in_=ss[:]), reads=[ss], writes=[ss])
                    for qs in range(4):
                        S.op("dve", lambda: v.scalar_tensor_tensor(out=ab_[:, qs, :], in0=o_[qs][:], scalar=ss[:, qs:qs + 1], in1=gsub[:],
                                                                   op0=ALU.mult, op1=ALU.mult), reads=[o_[qs], ss, gsub], writes=[ab_])
                    tok0 = b * SEQ + j * 512
                    S.dma("sp", T["at_ao"].t.ap()[tok0:tok0 + 512, h * 128:(h + 1) * 128].rearrange("(q p) c -> p q c", p=128), ab_[:], reads=[ab_])
            S.barrier()
            S.es = old
        with ExitStack() as es2:
            old = S.es
            S.es = es2
            wout = S.sb("at_wout", [128, 8, D], BF16)
            self.load_w_bf16(wout, T["attn_w_out"].t.ap()[jl], D)
            gt = S.sb("at_g", [128, D], F32)
            bt = S.sb("at_b", [128, D], F32)
            S.dma("sp", gt[:], T["ln_g"].t.ap()[l, 0:1, :].partition_broadcast(128), writes=[gt])
            S.dma("sp", bt[:], T["ln_b"].t.ap()[l, 0:1, :].partition_broadcast(128), writes=[bt])
            ao = [S.sb(f"at_ao{i}", [128, D], BF16) for i in range(3)]
            xr = [S.sb(f"at_xr{i}", [128, D], F32) for i in range(3)]
            pT = [S.ps(f"at_pTc{i}", [128, 8, 128], BF16) for i in range(1)] * 2
            R = self.router_alloc(l, T) if self.fuse_router else None
            rxb = [S.sb(f"at_rxb{i}", [128, D], BF16) for i in range(2)]
            aoT = [S.sb(f"at_aoT{i}", [128, 8, 128], BF16) for i in range(2)]
            py = [[S.ps(f"at_py{i}_{n}", [128, 512], F32) for n in range(2)] for i in range(2)]
            z1 = [S.sb(f"at_z1{i}", [128, D], F32) for i in range(2)]
            xo = [S.sb(f"at_xo{i}", [128, D], F32) for i in range(2)]
            st = [S.sb(f"at_st{i}", [128, 2, 6], F32) for i in range(2)]
            mv = [S.sb(f"at_mv{i}", [128, 2], F32) for i in range(2)]
            rstd = [S.sb(f"at_rstd{i}", [128, 1], F32) for i in range(2)]
            pend = []

            def ld(t):
                S.dma("sp", ao[t % 3][:], T["at_ao"].t.ap()[t * 128:(t + 1) * 128, :], writes=[ao[t % 3]])
                S.dma("sp", xr[t % 3][:], xin.t.ap()[t * 128:(t + 1) * 128, :], writes=[xr[t % 3]])
            ld(0)
            ld(1)
            for t in range(NT):
                if t + 2 < NT:
                    ld(t + 2)
                i = t % 2
                for c in range(8):
                    S.op("pe", lambda: pe.transpose(out=pT[i][:, c, :], in_=ao[t % 3][:, c * 128:(c + 1) * 128], identity=self.ident[:]),
                         reads=[ao[t % 3], self.ident], writes=[pT[i]])
                S.op("act", lambda: a.copy(out=aoT[i][:], in_=pT[i][:]), reads=[pT[i]], writes=[aoT[i]])
                for n in range(2):
                    for c in range(8):
                        S.op("pe", lambda: pe.matmul(py[i][n][:], lhsT=aoT[i][:, c, :], rhs=wout[:, c, n * 512:(n + 1) * 512], start=(c == 0), stop=(c == 7)),
                             reads=[aoT[i], wout], writes=[py[i][n]])
                    S.op("dve", lambda: v.scalar_tensor_tensor(out=z1[i][:, n * 512:(n + 1) * 512], in0=xr[t % 3][:, n * 512:(n + 1) * 512], scalar=ALPHA,
                                                               in1=py[i][n][:], op0=ALU.mult, op1=ALU.add), reads=[xr[t % 3], py[i][n]], writes=[z1[i]])
                self.ln_a(z1[i], st[i], mv[i], rstd[i])

                def fin(i=i, t=t):
                    self.ln_b(z1[i], xo[i], gt, bt, mv[i], rstd[i])
                    S.dma("sp", xout.t.ap()[t * 128:(t + 1) * 128, :], xo[i][:], reads=[xo[i]])
                    if R is not None:
                        self.route_and_stage(t, xo[i], R, rxb[i], T)
                if pend:
                    pend.pop()()
                pend.append(fin)
            pend.pop()()
            S.barrier()
            S.es = old


def _rel_bucket_np(rel):
    try:
        import jax
        import jax.numpy as jnp
        cpu = jax.devices("cpu")[0]
        with jax.default_device(cpu):
            r = jnp.asarray(rel, dtype=jnp.int32)
            nb = 16
            max_exact = 8
            ret = jnp.where(r > 0, nb, 0)
            n = jnp.abs(r)
            nf = jnp.maximum(n, 1).astype(jnp.float32)
            large = max_exact + (jnp.log(nf / max_exact) / math.log(128 / max_exact) * (nb - max_exact)).astype(jnp.int32)
            large = jnp.minimum(large, nb - 1)
            return np.asarray(ret + jnp.where(n < max_exact, n, large))
    except Exception:
        rel = np.asarray(rel, dtype=np.int32)
        nb, max_exact = 16, 8
        ret = np.where(rel > 0, nb, 0)
        n = np.abs(rel)
        nf = np.maximum(n, 1).astype(np.float32)
        large = max_exact + (np.log(nf / np.float32(max_exact)) / np.float32(math.log(128 / max_exact)) * np.float32(nb - max_exact)).astype(np.int32)
        large = np.minimum(large, nb - 1)
        return ret + np.where(n < max_exact, n, large)


def bias_tables(rel_bias):
    kk = np.arange(128)[:, None]
    c = np.arange(1152)[None, :]
    bucket = _rel_bucket_np(kk - c + 512)
    biasT = np.ascontiguousarray(np.transpose(rel_bias[bucket], (2, 0, 1))).astype(np.float32)
    cb = np.stack([rel_bias[15], rel_bias[31]], axis=1).reshape(-1)
    cbias = np.ascontiguousarray(np.broadcast_to(cb[None, :], (128, 16))).astype(np.float32)
    return biasT, cbias


def build_program():
    nc = bass.Bass("TRN2", target_bir_lowering=False)
    with ExitStack() as es:
        k = K(nc, es)
        k.nl = DEPTH
        k.fuse_router = False
        S = k.S
        T = {}
        x = S.dram("x", [NTOK, D], F32, kind="ExternalInput")
        for nm, shp in (("attn_w_in", [2, D, 3 * D]), ("attn_w_out", [2, D, D]), ("attn_lambda", [2, 4, 64]), ("attn_subln", [2, 128]),
                        ("biasT", [8, 128, 1152]), ("cbias", [128, 16]),
                        ("conv_w_in", [2, D, 3 * D]), ("conv_w", [2, 3, D]), ("conv_w_out", [2, D, D]),
                        ("router_w", [DEPTH, D, E]), ("router_b", [DEPTH, E]),
                        ("w1", [DEPTH * E * D, 2 * D]), ("b1", [DEPTH * E, 2 * D]), ("w2", [DEPTH * E * D, D]), ("b2", [DEPTH * E, D]),
                        ("ln_g", [DEPTH, 2, D]), ("ln_b", [DEPTH, 2, D])):
            T[nm] = S.dram(nm, shp, F32, kind="ExternalInput")
        out = S.dram("out", [NTOK, D], F32, kind="ExternalOutput")
        T["x1"] = S.dram("x1", [NTOK, D], F32)
        T["x1b"] = S.dram("x1b", [NTOK, D], BF16)
        T["xs"] = S.dram("xs", [PROWS, D], BF16)
        T["ys"] = S.dram("ys", [PROWS, D], BF16)
        T["cv_gb"] = S.dram("cv_gb", [D, NTOK], F32)
        T["cv_z"] = S.dram("cv_z", [D, NTOK], F32)
        T["at_qk"] = S.dram("at_qk", [16, 128, NTOK], BF16)
        T["at_v"] = S.dram("at_v", [NTOK, D], BF16)
        T["at_ao"] = S.dram("at_ao", [NTOK, D], BF16)
        T["w1c"] = S.dram("w1c", [E * 128, 8 * 2 * D], BF16)
        T["w2c"] = S.dram("w2c", [E * 128, 8 * D], BF16)
        T["b1c"] = S.dram("b1c", [E, 2 * D], BF16)
        T["b2c"] = S.dram("b2c", [E, D], BF16)
        xres = S.dram("xres", [NTOK, D], F32)
        k.consts()
        with ExitStack() as es2:
            old = S.es
            S.es = es2
            zt = S.sb("zfill", [128, 8 * D], BF16)
            S.op("pool", lambda: k.g.memset(zt[:], 0.0), writes=[zt])
            rows_per = 128 * 8
            for r0 in range(0, PROWS, rows_per):
                S.dma("sp", T["xs"].t.ap()[r0:r0 + rows_per, :].rearrange("(p a) d -> p (a d)", p=128), zt[:], reads=[zt])
            S.barrier()
            S.es = old
        for l in range(DEPTH):
            xin = x if l == 0 else xres
            xo = out if l == DEPTH - 1 else xres
            hook = (lambda l=l: k.convert_weights(l, T))
            if l % 2 == 0:
                k.attn_layer(l // 2, l, T, xin, T["x1"], hook=hook)
            else:
                k.conv_layer(l // 2, l, T, xin, T["x1"], hook=hook)
            k.moe_layer(l, T, xo)
        S.barrier()
    return nc


_NC_CACHE = {}


def kernel(x, rel_bias, attn_w_in, attn_lambda, attn_subln, attn_w_out, conv_w_in, conv_w, conv_w_out,
           router_w, router_b, w1, b1, w2, b2, ln_g, ln_b):
    f32 = lambda a_: np.ascontiguousarray(np.asarray(a_, dtype=np.float32))
    x = f32(x)
    B = x.shape[0]
    ncores = 8
    per = B // ncores
    biasT, cbias = bias_tables(f32(rel_bias))
    shared = {
        "attn_w_in": f32(attn_w_in), "attn_w_out": f32(attn_w_out), "attn_lambda": f32(attn_lambda), "attn_subln": f32(attn_subln),
        "biasT": biasT, "cbias": cbias,
        "conv_w_in": f32(conv_w_in), "conv_w": f32(conv_w), "conv_w_out": f32(conv_w_out),
        "router_w": f32(router_w), "router_b": f32(router_b),
        "w1": f32(w1).reshape(DEPTH * E * D, 2 * D), "b1": f32(b1).reshape(DEPTH * E, 2 * D),
        "w2": f32(w2).reshape(DEPTH * E * D, D), "b2": f32(b2).reshape(DEPTH * E, D),
        "ln_g": f32(ln_g), "ln_b": f32(ln_b),
    }
    if "nc" not in _NC_CACHE:
        _NC_CACHE["nc"] = build_program()
    nc = _NC_CACHE["nc"]
    in_maps = []
    for c in range(ncores):
        m = dict(shared)
        m["x"] = x[c * per:(c + 1) * per].reshape(NTOK, D)
        in_maps.append(m)
    res = run_bass_kernel_spmd(nc, in_maps, core_ids=list(range(ncores)))
    outs = [np.asarray(r["out"]).reshape(per, SEQ, D) for r in res.results]
    return np.concatenate(outs, axis=0).astype(np.float32)
```

```python
import math
import numpy as np
import concourse.bass as bass
import concourse.mybir as mybir
from concourse.bass_utils import run_bass_kernel_spmd
from contextlib import ExitStack

F32 = mybir.dt.float32
BF16 = mybir.dt.bfloat16
I32 = mybir.dt.int32
AF = mybir.ActivationFunctionType
ALU = mybir.AluOpType
AX = mybir.AxisListType

D = 1024
NTOK = 8192
NT = NTOK // 128
SEQ = 4096
E = 32
BK = 128
NB = (NTOK * 4 + E * (BK - 1) + BK - 1) // BK
PROWS = NB * BK
BIG = 65536.0
BIGI = float(1 << 22)
ALPHA = 8.0 ** 0.25
LN_EPS = 1e-5
DEPTH = 4


class Buf:
    __slots__ = ("t", "w", "r")

    def __init__(self, t):
        self.t = t
        self.w = None
        self.r = {}

    def __getitem__(self, idx):
        return self.t[idx]


class Sched:
    def __init__(self, nc, es, ndma=(("sp", 16), ("pool", 8), ("act", 4), ("bg", 6))):
        self.nc = nc
        self.es = es
        self.eng = {"pe": nc.tensor, "act": nc.scalar, "dve": nc.vector,
                    "pool": nc.gpsimd, "sp": nc.sync}
        self.csem = {k: es.enter_context(nc.semaphore(f"c_{k}"))
                     for k in ("pe", "act", "dve", "pool")}
        self.ccnt = {k: 0 for k in self.csem}
        self.dsem = {q: [es.enter_context(nc.semaphore(f"d_{q}{i}")) for i in range(n)]
                     for q, n in ndma}
        self.dcnt = {q: [0] * len(v) for q, v in self.dsem.items()}
        self.dnext = {q: 0 for q in self.dsem}
        self.seen = {k: {} for k in self.eng}
        self.qeng = {"sp": "sp", "pool": "pool", "act": "act", "bg": "pool"}
        self.nwait = 0
        self.ninst = 0

    def _uid(self, name):
        self.uid = getattr(self, "uid", 0) + 1
        return f"{name}_{self.uid}"

    def sb(self, name, shape, dt):
        return Buf(self.es.enter_context(self.nc.sbuf_tensor(self._uid(name), list(shape), dt)))

    def ps(self, name, shape, dt=F32):
        return Buf(self.es.enter_context(self.nc.psum_tensor(self._uid(name), list(shape), dt)))

    def dram(self, name, shape, dt, kind="Internal"):
        return Buf(self.nc.dram_tensor(name, list(shape), dt, kind=kind))

    def _wait(self, e, ev):
        key, sem, val = ev
        if e == "pe" and key == "pe":
            return
        if self.seen[e].get(key, 0) < val:
            self.eng[e].wait_ge(sem, val)
            self.seen[e][key] = val
            self.nwait += 1

    def _deps(self, e, reads, writes):
        for b in reads:
            if b.w is not None:
                self._wait(e, b.w)
        for b in writes:
            if b.w is not None:
                self._wait(e, b.w)
            for ev in b.r.values():
                self._wait(e, ev)

    def _record(self, ev, reads, writes):
        for b in reads:
            b.r[ev[0]] = ev
        for b in writes:
            b.w = ev
            b.r = {}

    def op(self, e, fn, reads=(), writes=()):
        self._deps(e, reads, writes)
        inst = fn()
        self.ccnt[e] += 1
        inst.then_inc(self.csem[e], 1)
        self._record((e, self.csem[e], self.ccnt[e]), reads, writes)
        self.ninst += 1
        return inst

    def dma(self, q, out, in_, reads=(), writes=(), indirect=None, **kw):
        e = self.qeng[q]
        self._deps(e, reads, writes)
        i = self.dnext[q]
        self.dnext[q] = (i + 1) % len(self.dsem[q])
        sem = self.dsem[q][i]
        key = ("d", q, i)
        prev = self.dcnt[q][i]
        self._wait(e, (key, sem, prev))
        if indirect is not None:
            inst = self.eng[e].indirect_dma_start(out=out, in_=in_, **indirect, **kw)
        else:
            inst = self.eng[e].dma_start(out=out, in_=in_, **kw)
        inst.then_inc(sem, 16)
        self.dcnt[q][i] = prev + 16
        self._record((key, sem, prev + 16), reads, writes)
        self.ninst += 1
        return inst

    def wait_bg(self, engines=("pool",)):
        for i, sm in enumerate(self.dsem["bg"]):
            if self.dcnt["bg"][i] > 0:
                for e in engines:
                    self._wait(e, (("d", "bg", i), sm, self.dcnt["bg"][i]))

    def barrier(self):
        evs = [(k, self.csem[k], self.ccnt[k]) for k in self.csem if self.ccnt[k] > 0]
        for q in self.dsem:
            if q == "bg":
                continue
            for i, s in enumerate(self.dsem[q]):
                if self.dcnt[q][i] > 0:
                    evs.append((("d", q, i), s, self.dcnt[q][i]))
        for e in self.eng:
            for ev in evs:
                if ev[0] == e:
                    continue
                self._wait(e, ev)


def gather(ix_ap):
    return dict(out_offset=None, in_offset=bass.IndirectOffsetOnAxis(ap=ix_ap, axis=0))


def scatter(ix_ap):
    return dict(out_offset=bass.IndirectOffsetOnAxis(ap=ix_ap, axis=0), in_offset=None)


class K:
    def __init__(self, nc, es):
        self.nc = nc
        self.es = es
        self.S = Sched(nc, es)
        self.v = nc.vector
        self.g = nc.gpsimd
        self.a = nc.scalar
        self.pe = nc.tensor
        self.nl = DEPTH
        self.dbg = {}
        self.fuse_router = False

    def consts(self):
        S, nc = self.S, self.nc
        self.identf = S.sb("identf", [128, 128], F32)
        self.ident = S.sb("ident", [128, 128], BF16)
        self.onesb = S.sb("onesb", [128, 128], BF16)
        self.onesf = S.sb("onesf", [128, 128], F32)
        self.lsb = S.sb("lsb", [128, 128], BF16)
        tmp = S.sb("ctmp", [128, 128], F32)
        S.op("pool", lambda: self.g.memset(self.onesf[:], 1.0), writes=[self.onesf])
        S.op("pool", lambda: self.g.memset(self.identf[:], 1.0), writes=[self.identf])
        S.op("pool", lambda: self.g.affine_select(out=self.identf[:], in_=self.identf[:], pattern=[[-1, 128]],
                                                  compare_op=ALU.is_equal, fill=0.0, base=0, channel_multiplier=1),
             reads=[self.identf], writes=[self.identf])
        S.op("pool", lambda: self.g.memset(tmp[:], 1.0), writes=[tmp])
        S.op("pool", lambda: self.g.affine_select(out=tmp[:], in_=tmp[:], pattern=[[1, 128]],
                                                  compare_op=ALU.is_gt, fill=0.0, base=0, channel_multiplier=-1),
             reads=[tmp], writes=[tmp])
        S.op("dve", lambda: self.v.tensor_copy(out=self.ident[:], in_=self.identf[:]), reads=[self.identf], writes=[self.ident])
        S.op("dve", lambda: self.v.tensor_copy(out=self.onesb[:], in_=self.onesf[:]), reads=[self.onesf], writes=[self.onesb])
        self.inv128 = S.sb("inv128", [128, 128], BF16)
        S.op("dve", lambda: self.v.tensor_scalar(out=self.inv128[:], in0=self.onesf[:], scalar1=1.0 / 128.0, scalar2=None, op0=ALU.mult),
             reads=[self.onesf], writes=[self.inv128])
        S.op("dve", lambda: self.v.tensor_copy(out=self.lsb[:], in_=tmp[:]), reads=[tmp], writes=[self.lsb])
        self.kp = S.sb("kp", [128, 8], F32)
        S.op("pool", lambda: self.g.iota(out=self.kp[:], pattern=[[128, 8]], base=0, channel_multiplier=1,
                                         allow_small_or_imprecise_dtypes=True), writes=[self.kp])
        self.bvals = S.sb("bvals", [128, NB], F32)
        S.op("pool", lambda: self.g.iota(out=self.bvals[:], pattern=[[BK, NB]], base=0, channel_multiplier=0,
                                         allow_small_or_imprecise_dtypes=True), writes=[self.bvals])
        self.reg_wrow = nc.gpsimd.to_reg(E * 128 - 1)
        self.reg_brow = nc.gpsimd.to_reg(E - 1)
        self.reg_prow = nc.gpsimd.to_reg(PROWS - 1)
        self.mask_all = S.sb("mask_all", [128, NT, E], BF16)
        self.gates_all = S.sb("gates_all", [128, NT, E], F32)
        self.idx_all = S.sb("idx_all", [128, NT, 4], I32)
        self.gsel_all = S.sb("gsel_all", [128, NT, 4], F32)
        self.widx = S.sb("widx", [128, NB], I32)
        self.bidx = S.sb("bidx", [128, NB], I32)

    def ln_a(self, z, st, mv, rstd):
        S, v = self.S, self.v
        for c in range(2):
            S.op("dve", lambda: v.bn_stats(out=st[:, c, :], in_=z[:, c * 512:(c + 1) * 512]), reads=[z], writes=[st])
        S.op("dve", lambda: v.bn_aggr(out=mv[:], in_=st[:]), reads=[st], writes=[mv])
        S.op("dve", lambda: v.tensor_scalar(out=rstd[:], in0=mv[:, 1:2], scalar1=LN_EPS, scalar2=None, op0=ALU.add),
             reads=[mv], writes=[rstd])
        S.op("act", lambda: self.a.sqrt(out=rstd[:], in_=rstd[:]), reads=[rstd], writes=[rstd])

    def ln_b(self, z, out, gt, bt, mv, rstd):
        S, v = self.S, self.v
        S.op("dve", lambda: v.reciprocal(out=rstd[:], in_=rstd[:]), reads=[rstd], writes=[rstd])
        S.op("dve", lambda: v.tensor_scalar(out=out[:], in0=z[:], scalar1=mv[:, 0:1], scalar2=rstd[:, 0:1],
                                            op0=ALU.subtract, op1=ALU.mult), reads=[z, mv, rstd], writes=[out])
        S.op("dve", lambda: v.tensor_tensor(out=out[:], in0=out[:], in1=gt[:], op=ALU.mult), reads=[out, gt], writes=[out])
        S.op("dve", lambda: v.tensor_tensor(out=out[:], in0=out[:], in1=bt[:], op=ALU.add), reads=[out, bt], writes=[out])

    def layer_norm(self, z, out, gt, bt, st, mv, rstd, eng2="dve"):
        self.ln_a(z, st, mv, rstd)
        self.ln_b(z, out, gt, bt, mv, rstd)

    def router_alloc(self, l, T):
        S = self.S
        R = {}
        R["rw"] = S.sb("rw", [128, 8, E], F32)
        R["rb"] = S.sb("rb", [1, E], F32)
        S.dma("sp", R["rw"][:], T["router_w"].t.ap()[l].rearrange("(k p) e -> p k e", p=128), reads=[], writes=[R["rw"]])
        S.dma("sp", R["rb"][:], T["router_b"].t.ap()[l:l + 1, :], reads=[], writes=[R["rb"]])
        R["pTf"] = [S.ps(f"pTf{i}", [128, 4, 128], F32) for i in range(2)]
        R["plg"] = S.ps("plg", [128, E], F32)
        R["x1T"] = S.sb("x1T", [128, 8, 128], F32)
        R["lg"] = S.sb("lg", [128, E], F32)
        R["t8"] = S.sb("t8", [128, 8], F32)
        R["nmx"] = S.sb("nmx", [128, 1], F32)
        R["msk"] = S.sb("msk", [128, E], F32)
        R["ex"] = S.sb("ex", [128, E], F32)
        R["ssum"] = S.sb("ssum", [128, 1], F32)
        return R

    def router_tile(self, j, x1s, R):
        S, v = self.S, self.v
        for h in range(2):
            for k in range(4):
                kk = h * 4 + k
                S.op("pe", lambda: self.pe.transpose(out=R["pTf"][h][:, k, :], in_=x1s[:, kk * 128:(kk + 1) * 128],
                                                     identity=self.identf[:]), reads=[x1s, self.identf], writes=[R["pTf"][h]])
            S.op("act", lambda: self.a.copy(out=R["x1T"][:, h * 4:(h + 1) * 4, :], in_=R["pTf"][h][:]),
                 reads=[R["pTf"][h]], writes=[R["x1T"]])
        for k in range(8):
            S.op("pe", lambda: self.pe.matmul(R["plg"][:], lhsT=R["x1T"][:, k, :], rhs=R["rw"][:, k, :], start=(k == 0), stop=False),
                 reads=[R["x1T"], R["rw"]], writes=[R["plg"]])
        S.op("pe", lambda: self.pe.matmul(R["plg"][:], lhsT=self.onesf[0:1, :], rhs=R["rb"][0:1, :], start=False, stop=True),
             reads=[self.onesf, R["rb"]], writes=[R["plg"]])
        S.op("dve", lambda: v.tensor_copy(out=R["lg"][:], in_=R["plg"][:]), reads=[R["plg"]], writes=[R["lg"]])
        S.op("dve", lambda: v.max(out=R["t8"][:], in_=R["lg"][:]), reads=[R["lg"]], writes=[R["t8"]])
        S.op("dve", lambda: v.tensor_scalar(out=R["nmx"][:], in0=R["t8"][:, 0:1], scalar1=-1.0, scalar2=None, op0=ALU.mult),
             reads=[R["t8"]], writes=[R["nmx"]])
        S.op("dve", lambda: v.tensor_scalar(out=R["msk"][:], in0=R["lg"][:], scalar1=R["t8"][:, 3:4], scalar2=None, op0=ALU.is_ge),
             reads=[R["lg"], R["t8"]], writes=[R["msk"]])
        S.op("act", lambda: self.a.activation(out=R["ex"][:], in_=R["lg"][:], func=AF.Exp, bias=R["nmx"][:, 0:1], scale=1.0),
             reads=[R["lg"], R["nmx"]], writes=[R["ex"]])
        S.op("dve", lambda: v.tensor_tensor(out=R["ex"][:], in0=R["ex"][:], in1=R["msk"][:], op=ALU.mult),
             reads=[R["ex"], R["msk"]], writes=[R["ex"]])
        S.op("dve", lambda: v.reduce_sum(out=R["ssum"][:], in_=R["ex"][:], axis=AX.X), reads=[R["ex"]], writes=[R["ssum"]])
        S.op("dve", lambda: v.reciprocal(out=R["ssum"][:], in_=R["ssum"][:]), reads=[R["ssum"]], writes=[R["ssum"]])
        S.op("dve", lambda: v.tensor_scalar(out=self.gates_all[:, j, :], in0=R["ex"][:], scalar1=R["ssum"][:, 0:1], scalar2=None, op0=ALU.mult),
             reads=[R["ex"], R["ssum"]], writes=[self.gates_all])
        S.op("dve", lambda: v.tensor_copy(out=self.mask_all[:, j, :], in_=R["msk"][:]), reads=[R["msk"]], writes=[self.mask_all])

    def route_and_stage(self, j, x1s, R, xb, T):
        S = self.S
        S.op("act", lambda: self.a.copy(out=xb[:], in_=x1s[:]), reads=[x1s], writes=[xb])
        S.dma("sp", T["x1b"].t.ap()[j * 128:(j + 1) * 128, :], xb[:], reads=[xb])
        self.router_tile(j, x1s, R)

    def routing_tables(self, l):
        S, v, g = self.S, self.v, self.g
        with ExitStack() as es2:
            old = S.es
            S.es = es2
            rank = S.sb("rank", [128, NT, E], F32)
            tot = S.sb("tot", [128, NT, E], F32)
            pref = S.sb("pref", [128, NT, E], F32)
            val = S.sb("val", [128, NT, E], F32)
            cnt = S.sb("cnt", [128, E], F32)
            pad = S.sb("pad", [128, E], F32)
            pst = S.sb("pst", [128, E], F32)
            pend = S.sb("pend", [128, E], F32)
            t8a = S.sb("t8a", [128, NT, 8], F32)
            posk = S.sb("posk", [128, NT, 4], F32)
            sel = S.sb("sel", [128, NT, E], F32)
            blke = S.sb("blke", [128, NB], F32)
            sw = S.sb("sw", [128, NB], F32)
            base = S.sb("base", [128, NB], F32)
            pw = [S.ps(f"pw{i}", [128, 512], F32) for i in range(2)]
            pt = [S.ps(f"ptt{i}", [128, 512], F32) for i in range(2)]
            for c in range(4):
                rhs = self.mask_all[:, 16 * c:16 * c + 16, :].rearrange("p a b -> p (a b)")
                S.op("pe", lambda: self.pe.matmul(pw[c % 2][:], lhsT=self.lsb[:], rhs=rhs, start=True, stop=True),
                     reads=[self.lsb, self.mask_all], writes=[pw[c % 2]])
                S.op("pe", lambda: self.pe.matmul(pt[c % 2][:], lhsT=self.onesb[:], rhs=rhs, start=True, stop=True),
                     reads=[self.onesb, self.mask_all], writes=[pt[c % 2]])
                S.op("dve", lambda: v.tensor_copy(out=rank[:, 16 * c:16 * c + 16, :].rearrange("p a b -> p (a b)"), in_=pw[c % 2][:]),
                     reads=[pw[c % 2]], writes=[rank])
                S.op("act", lambda: self.a.copy(out=tot[:, 16 * c:16 * c + 16, :].rearrange("p a b -> p (a b)"), in_=pt[c % 2][:]),
                     reads=[pt[c % 2]], writes=[tot])
            S.op("dve", lambda: v.memset(pref[:, 0, :], 0.0), writes=[pref])
            for j in range(1, NT):
                S.op("dve", lambda: v.tensor_tensor(out=pref[:, j, :], in0=pref[:, j - 1, :], in1=tot[:, j - 1, :], op=ALU.add),
                     reads=[pref, tot], writes=[pref])
            S.op("dve", lambda: v.tensor_tensor(out=cnt[:], in0=pref[:, NT - 1, :], in1=tot[:, NT - 1, :], op=ALU.add),
                 reads=[pref, tot], writes=[cnt])
            cnti = S.sb("cnti", [128, E], I32)
            S.op("dve", lambda: v.tensor_copy(out=cnti[:], in_=cnt[:]), reads=[cnt], writes=[cnti])
            S.op("dve", lambda: v.tensor_scalar(out=cnti[:], in0=cnti[:], scalar1=BK - 1, scalar2=None, op0=ALU.add), reads=[cnti], writes=[cnti])
            S.op("dve", lambda: v.tensor_scalar(out=cnti[:], in0=cnti[:], scalar1=7, scalar2=7, op0=ALU.arith_shift_right, op1=ALU.logical_shift_left),
                 reads=[cnti], writes=[cnti])
            S.op("dve", lambda: v.tensor_copy(out=pad[:], in_=cnti[:]), reads=[cnti], writes=[pad])
            S.op("dve", lambda: v.memset(pst[:, 0:1], 0.0), writes=[pst])
            for e in range(1, E):
                S.op("dve", lambda: v.tensor_tensor(out=pst[:, e:e + 1], in0=pst[:, e - 1:e], in1=pad[:, e - 1:e], op=ALU.add),
                     reads=[pst, pad], writes=[pst])
            S.op("dve", lambda: v.tensor_tensor(out=pend[:], in0=pst[:], in1=pad[:], op=ALU.add), reads=[pst, pad], writes=[pend])
            S.op("dve", lambda: v.tensor_tensor(out=rank[:], in0=rank[:], in1=pref[:], op=ALU.add), reads=[rank, pref], writes=[rank])
            S.op("dve", lambda: v.tensor_tensor(out=rank[:], in0=rank[:], in1=pst[:, :].unsqueeze(1).to_broadcast([128, NT, E]), op=ALU.add),
                 reads=[rank, pst], writes=[rank])
            S.op("dve", lambda: v.tensor_scalar(out=val[:], in0=rank[:], scalar1=-1.0, scalar2=BIG, op0=ALU.mult, op1=ALU.add),
                 reads=[rank], writes=[val])
            S.op("dve", lambda: v.tensor_tensor(out=val[:], in0=val[:], in1=self.mask_all[:], op=ALU.mult),
                 reads=[val, self.mask_all], writes=[val])
            for j in range(NT):
                S.op("dve", lambda: v.max(out=t8a[:, j, :], in_=val[:, j, :]), reads=[val], writes=[t8a])
            S.op("dve", lambda: v.tensor_scalar(out=posk[:], in0=t8a[:, :, 0:4], scalar1=-1.0, scalar2=BIG, op0=ALU.mult, op1=ALU.add),
                 reads=[t8a], writes=[posk])
            S.op("dve", lambda: v.tensor_copy(out=self.idx_all[:], in_=posk[:]), reads=[posk], writes=[self.idx_all])
            for k in range(4):
                S.op("dve", lambda: v.tensor_tensor(out=sel[:], in0=val[:], in1=t8a[:, :, k:k + 1].to_broadcast([128, NT, E]), op=ALU.is_equal),
                     reads=[val, t8a], writes=[sel])
                S.op("dve", lambda: v.tensor_tensor(out=sel[:], in0=sel[:], in1=self.gates_all[:], op=ALU.mult),
                     reads=[sel, self.gates_all], writes=[sel])
                S.op("dve", lambda: v.tensor_reduce(out=self.gsel_all[:, :, k:k + 1], in_=sel[:], axis=AX.X, op=ALU.add),
                     reads=[sel], writes=[self.gsel_all])
            S.op("dve", lambda: v.memset(blke[:], 0.0), writes=[blke])
            for e in range(E):
                S.op("dve", lambda: v.scalar_tensor_tensor(out=blke[:], in0=self.bvals[:], scalar=pend[:, e:e + 1], in1=blke[:],
                                                           op0=ALU.is_ge, op1=ALU.add), reads=[self.bvals, pend, blke], writes=[blke])
            S.op("dve", lambda: v.tensor_scalar(out=blke[:], in0=blke[:], scalar1=float(E - 1), scalar2=None, op0=ALU.min),
                 reads=[blke], writes=[blke])
            S.op("dve", lambda: v.memset(sw[:, 0:1], 1.0), writes=[sw])
            S.op("dve", lambda: v.tensor_tensor(out=sw[:, 1:NB], in0=blke[:, 1:NB], in1=blke[:, 0:NB - 1], op=ALU.not_equal),
                 reads=[blke], writes=[sw])
            S.op("dve", lambda: v.tensor_scalar(out=sw[:], in0=sw[:], scalar1=-BIGI, scalar2=BIGI, op0=ALU.mult, op1=ALU.add),
                 reads=[sw], writes=[sw])
            S.op("dve", lambda: v.tensor_scalar(out=base[:], in0=blke[:], scalar1=128.0, scalar2=self.kp[:, 0:1], op0=ALU.mult, op1=ALU.add),
                 reads=[blke, self.kp], writes=[base])
            S.op("dve", lambda: v.tensor_tensor(out=base[:], in0=base[:], in1=sw[:], op=ALU.add), reads=[base, sw], writes=[base])
            S.op("dve", lambda: v.tensor_copy(out=self.widx[:], in_=base[:]), reads=[base], writes=[self.widx])
            S.op("dve", lambda: v.tensor_tensor(out=base[:], in0=blke[:], in1=sw[:], op=ALU.add), reads=[blke, sw], writes=[base])
            S.op("dve", lambda: v.tensor_copy(out=self.bidx[:], in_=base[:]), reads=[base], writes=[self.bidx])
            for nm, bf in (("gates_all", self.gates_all), ("idx_all", self.idx_all), ("gsel_all", self.gsel_all),
                           ("widx", self.widx), ("bidx", self.bidx), ("cnt", cnt), ("pst", pst), ("blke", blke)):
                if nm in self.dbg:
                    S.dma("sp", self.dbg[nm].t.ap(), bf[:], reads=[bf])
            S.barrier()
            S.es = old

    def dispatch(self, T):
        S = self.S
        with ExitStack() as es2:
            old = S.es
            S.es = es2
            xb = [S.sb(f"dxb{i}", [128, D], BF16) for i in range(3)]
            for j in range(NT):
                b = xb[j % 3]
                S.dma("sp", b[:], T["x1b"].t.ap()[j * 128:(j + 1) * 128, :], reads=[], writes=[b])
                for k in range(4):
                    S.dma("pool", T["xs"].t.ap(), b[:], reads=[b, self.idx_all], writes=[],
                          indirect=dict(**scatter(self.idx_all[:, j, k:k + 1]), bounds_check=self.reg_prow, oob_is_err=False))
            S.barrier()
            S.es = old

    def convert_weights(self, l, T):
        S = self.S
        for e in range(E):
            src = T["w1"].t.ap()[(l * E + e) * D:(l * E + e + 1) * D, :].rearrange("(k p) n -> p k n", p=128)
            dst = T["w1c"].t.ap()[e * 128:(e + 1) * 128, :].rearrange("p (k n) -> p k n", k=8)
            for hf in range(2):
                S.dma("bg", dst[:, hf * 4:(hf + 1) * 4, :], src[:, hf * 4:(hf + 1) * 4, :])
            src = T["w2"].t.ap()[(l * E + e) * D:(l * E + e + 1) * D, :].rearrange("(k p) n -> p k n", p=128)
            dst = T["w2c"].t.ap()[e * 128:(e + 1) * 128, :].rearrange("p (k n) -> p k n", k=8)
            for hf in range(2):
                S.dma("bg", dst[:, hf * 4:(hf + 1) * 4, :], src[:, hf * 4:(hf + 1) * 4, :])
        S.dma("bg", T["b1c"].t.ap(), T["b1"].t.ap()[l * E:(l + 1) * E, :])

    def experts(self, l, T):
        S, v, g, a, pe = self.S, self.v, self.g, self.a, self.pe
        with ExitStack() as es2:
            old = S.es
            S.es = es2
            S.wait_bg()
            W1 = S.sb("W1", [128, 8 * 2 * D], BF16)
            W2 = S.sb("W2", [128, 8 * D], BF16)
            B1 = S.sb("B1", [128, 2 * D], BF16)
            xsb = [S.sb(f"xsb{i}", [128, D], BF16) for i in range(3)]
            XT = [S.sb(f"XT{i}", [128, 8, 128], BF16) for i in range(2)]
            AT = [S.sb(f"AT{i}", [128, 8, 128], BF16) for i in range(2)]
            actb = [S.sb(f"actb{i}", [128, D], BF16) for i in range(2)]
            gl = [S.sb(f"gl{i}", [128, 256], F32) for i in range(2)]
            sg = [S.sb(f"sg{i}", [128, 256], F32) for i in range(2)]
            ln = [S.sb(f"ln{i}", [128, 256], F32) for i in range(2)]
            hb = [S.sb(f"hb{i}", [128, 512], F32) for i in range(2)]
            ysb = [S.sb(f"ysb{i}", [128, D], BF16) for i in range(2)]
            pT1 = S.ps("pT1", [128, 8, 128], BF16)
            pT2 = S.ps("pT2", [128, 8, 128], BF16)
            ph = [S.ps(f"ph{n}", [128, 512], F32) for n in range(4)]
            py = [S.ps(f"py{n}", [128, 512], F32) for n in range(2)]
            PRE = 2
            for b in range(min(PRE, NB)):
                S.dma("sp", xsb[b % 3][:], T["xs"].t.ap()[b * BK:(b + 1) * BK, :], reads=[], writes=[xsb[b % 3]])

            def t1(b):
                xs_ = xsb[b % 3]
                for k in range(8):
                    S.op("pe", lambda: pe.transpose(out=pT1[:, k, :], in_=xs_[:, k * 128:(k + 1) * 128], identity=self.ident[:]),
                         reads=[xs_, self.ident], writes=[pT1])
                S.op("act", lambda: a.copy(out=XT[b % 2][:], in_=pT1[:]), reads=[pT1], writes=[XT[b % 2]])

            def p1(b):
                xt = XT[b % 2]
                ab = actb[b % 2]
                for n in range(4):
                    for k in range(8):
                        S.op("pe", lambda: pe.matmul(ph[n][:], lhsT=xt[:, k, :], rhs=W1[:, k * 2048 + n * 512:k * 2048 + (n + 1) * 512], start=(k == 0), stop=False),
                             reads=[xt, W1], writes=[ph[n]])
                    S.op("pe", lambda: pe.matmul(ph[n][:], lhsT=self.inv128[:], rhs=B1[:, n * 512:(n + 1) * 512], start=False, stop=True),
                         reads=[self.inv128, B1], writes=[ph[n]])
                    i2 = n % 2
                    hv = ph[n][:].rearrange("p (c two) -> p c two", two=2)
                    S.op("dve", lambda: v.tensor_scalar(out=gl[i2][:], in0=hv[:, :, 0], scalar1=7.0, scalar2=None, op0=ALU.min),
                         reads=[ph[n]], writes=[gl[i2]])
                    S.op("dve", lambda: v.tensor_scalar(out=ln[i2][:], in0=hv[:, :, 1], scalar1=-7.0, scalar2=7.0, op0=ALU.max, op1=ALU.min),
                         reads=[ph[n]], writes=[ln[i2]])
                    S.op("act", lambda: a.activation(out=sg[i2][:], in_=gl[i2][:], func=AF.Sigmoid, scale=1.702),
                         reads=[gl[i2]], writes=[sg[i2]])
                    S.op("act", lambda: a.activation(out=ln[i2][:], in_=ln[i2][:], func=AF.Identity, bias=1.0, scale=1.0),
                         reads=[ln[i2]], writes=[ln[i2]])
                    S.op("dve", lambda: v.tensor_tensor(out=sg[i2][:], in0=sg[i2][:], in1=gl[i2][:], op=ALU.mult),
                         reads=[sg[i2], gl[i2]], writes=[sg[i2]])
                    S.op("dve", lambda: v.tensor_tensor(out=ab[:, n * 256:(n + 1) * 256], in0=ln[i2][:], in1=sg[i2][:], op=ALU.mult),
                         reads=[ln[i2], sg[i2]], writes=[ab])

            def t2(b):
                ab = actb[b % 2]
                for k in range(8):
                    S.op("pe", lambda: pe.transpose(out=pT2[:, k, :], in_=ab[:, k * 128:(k + 1) * 128], identity=self.ident[:]),
                         reads=[ab, self.ident], writes=[pT2])
                S.op("act", lambda: a.copy(out=AT[b % 2][:], in_=pT2[:]), reads=[pT2], writes=[AT[b % 2]])

            def p2(b):
                at = AT[b % 2]
                y = ysb[b % 2]
                for n in range(2):
                    for k in range(8):
                        S.op("pe", lambda: pe.matmul(py[n][:], lhsT=at[:, k, :], rhs=W2[:, k * 1024 + n * 512:k * 1024 + (n + 1) * 512], start=(k == 0), stop=(k == 7)),
                             reads=[at, W2], writes=[py[n]])
                    if n == 0:
                        S.op("dve", lambda: v.tensor_copy(out=y[:, 0:512], in_=py[0][:]), reads=[py[0]], writes=[y])
                    else:
                        S.op("act", lambda: a.copy(out=y[:, 512:1024], in_=py[1][:]), reads=[py[1]], writes=[y])
                S.dma("sp", T["ys"].t.ap()[b * BK:(b + 1) * BK, :], y[:], reads=[y], writes=[])

            def g1(b):
                S.dma("pool", W1[:], T["w1c"].t.ap(), reads=[self.widx], writes=[W1],
                      indirect=dict(**gather(self.widx[:, b:b + 1]), bounds_check=self.reg_wrow, oob_is_err=False))
                S.dma("pool", B1[:], T["b1c"].t.ap(), reads=[self.bidx], writes=[B1],
                      indirect=dict(**gather(self.bidx[:, b:b + 1]), bounds_check=self.reg_brow, oob_is_err=False))

            def g2(b):
                S.dma("pool", W2[:], T["w2c"].t.ap(), reads=[self.widx], writes=[W2],
                      indirect=dict(**gather(self.widx[:, b:b + 1]), bounds_check=self.reg_wrow, oob_is_err=False))

            g1(0)
            t1(0)
            for b in range(NB):
                if b + PRE < NB:
                    bb = b + PRE
                    S.dma("sp", xsb[bb % 3][:], T["xs"].t.ap()[bb * BK:(bb + 1) * BK, :], reads=[], writes=[xsb[bb % 3]])
                p1(b)
                if b + 1 < NB:
                    g1(b + 1)
                if b >= 1:
                    p2(b - 1)
                g2(b)
                if b + 1 < NB:
                    t1(b + 1)
                t2(b)
            p2(NB - 1)
            S.barrier()
            S.es = old

    def combine(self, l, T, xout):
        S, v, g = self.S, self.v, self.g
        with ExitStack() as es2:
            old = S.es
            S.es = es2
            gt = S.sb("c_g", [128, D], F32)
            bt = S.sb("c_b", [128, D], F32)
            S.dma("sp", gt[:], T["ln_g"].t.ap()[l, 1:2, :].partition_broadcast(128), reads=[], writes=[gt])
            S.dma("sp", bt[:], T["ln_b"].t.ap()[l, 1:2, :].partition_broadcast(128), reads=[], writes=[bt])
            NBUF = 2
            yk = [[S.sb(f"yk{i}_{k}", [128, D], BF16) for k in range(4)] for i in range(NBUF)]
            dg = [[S.sb(f"dg{i}_{k}", [128, 128], BF16) for k in range(4)] for i in range(NBUF)]
            x1 = [S.sb(f"cx1_{i}", [128, D], F32) for i in range(NBUF)]
            acc = [S.sb(f"acc{i}", [128, D], F32) for i in range(NBUF)]
            xo = [S.sb(f"cxo{i}", [128, D], F32) for i in range(NBUF)]
            st = [S.sb(f"c_st{i}", [128, 2, 6], F32) for i in range(2)]
            mv = [S.sb(f"c_mv{i}", [128, 2], F32) for i in range(2)]
            rstd = [S.sb(f"c_rstd{i}", [128, 1], F32) for i in range(2)]
            b2f = S.sb("c_b2f", [E, D], F32)
            S.dma("sp", b2f[:], T["b2"].t.ap()[l * E:(l + 1) * E, :], writes=[b2f])
            b2s = S.sb("c_b2s", [E, D], BF16)
            S.op("dve", lambda: v.tensor_copy(out=b2s[:], in_=b2f[:]), reads=[b2f], writes=[b2s])
            pgT = S.ps("c_pgT", [E, 128], F32)
            gT = [S.sb(f"c_gT{i}", [E, 128], BF16) for i in range(2)]
            pb = [[S.ps(f"c_pb{i}_{n}", [128, 512], F32) for n in range(2)] for i in range(2)]

            def loads(j):
                i = j % NBUF
                S.dma("sp", x1[i][:], T["x1"].t.ap()[j * 128:(j + 1) * 128, :], reads=[], writes=[x1[i]])
                for k in range(4):
                    S.dma("pool", yk[i][k][:], T["ys"].t.ap(), reads=[self.idx_all], writes=[yk[i][k]],
                          indirect=dict(**gather(self.idx_all[:, j, k:k + 1]), bounds_check=self.reg_prow, oob_is_err=False))
            loads(0)
            for j in range(NT):
                i = j % NBUF
                if j + 1 < NT:
                    loads(j + 1)
                S.op("pe", lambda: self.pe.transpose(out=pgT[:], in_=self.gates_all[:, j, :], identity=self.identf[:]),
                     reads=[self.gates_all, self.identf], writes=[pgT])
                S.op("act", lambda: self.a.copy(out=gT[i][:], in_=pgT[:]), reads=[pgT], writes=[gT[i]])
                for k in range(4):
                    S.op("act", lambda: self.a.activation(out=dg[i][k][:], in_=self.ident[:], func=AF.Copy, scale=self.gsel_all[:, j, k:k + 1]),
                         reads=[self.ident, self.gsel_all], writes=[dg[i][k]])
                for n in range(2):
                    S.op("pe", lambda: self.pe.matmul(pb[i][n][:], lhsT=gT[i][:], rhs=b2s[:, n * 512:(n + 1) * 512], start=True, stop=False),
                         reads=[gT[i], b2s], writes=[pb[i][n]])
                    for k in range(4):
                        S.op("pe", lambda: self.pe.matmul(pb[i][n][:], lhsT=dg[i][k][:], rhs=yk[i][k][:, n * 512:(n + 1) * 512], start=False, stop=(k == 3)),
                             reads=[dg[i][k], yk[i][k]], writes=[pb[i][n]])
                    S.op("dve", lambda: v.scalar_tensor_tensor(out=acc[i][:, n * 512:(n + 1) * 512], in0=x1[i][:, n * 512:(n + 1) * 512], scalar=ALPHA,
                                                               in1=pb[i][n][:], op0=ALU.mult, op1=ALU.add),
                         reads=[x1[i], pb[i][n]], writes=[acc[i]])
                self.ln_a(acc[i], st[i], mv[i], rstd[i])
                if j >= 1:
                    ip = (j - 1) % NBUF
                    self.ln_b(acc[ip], xo[ip], gt, bt, mv[ip], rstd[ip])
                    S.dma("sp", xout.t.ap()[(j - 1) * 128:j * 128, :], xo[ip][:], reads=[xo[ip]], writes=[])
            ip = (NT - 1) % NBUF
            self.ln_b(acc[ip], xo[ip], gt, bt, mv[ip], rstd[ip])
            S.dma("sp", xout.t.ap()[(NT - 1) * 128:NT * 128, :], xo[ip][:], reads=[xo[ip]], writes=[])
            S.barrier()
            S.es = old

    def router_pass(self, l, T):
        S, v, g = self.S, self.v, self.g
        with ExitStack() as es2:
            old = S.es
            S.es = es2
            R = self.router_alloc(l, T)
            xs_ = [S.sb(f"rx{i}", [128, D], F32) for i in range(3)]
            xb = [S.sb(f"rxb{i}", [128, D], BF16) for i in range(2)]
            PRE = 2
            for j in range(min(PRE, NT)):
                S.dma("sp", xs_[j % 3][:], T["x1"].t.ap()[j * 128:(j + 1) * 128, :], writes=[xs_[j % 3]])
            for j in range(NT):
                if j + PRE < NT:
                    jj = j + PRE
                    S.dma("sp", xs_[jj % 3][:], T["x1"].t.ap()[jj * 128:(jj + 1) * 128, :], writes=[xs_[jj % 3]])
                x = xs_[j % 3]
                b = xb[j % 2]
                S.op("act", lambda: self.a.copy(out=b[:], in_=x[:]), reads=[x], writes=[b])
                S.dma("sp", T["x1b"].t.ap()[j * 128:(j + 1) * 128, :], b[:], reads=[b])
                self.router_tile(j, x, R)
            S.barrier()
            S.es = old

    def moe_layer(self, l, T, xout):
        if not self.fuse_router:
            self.router_pass(l, T)
        self.routing_tables(l)
        self.dispatch(T)
        self.experts(l, T)
        self.combine(l, T, xout)

    def load_w_bf16(self, dst, src_ap, ncols, col0=0, q="pool"):
        S = self.S
        v = src_ap.rearrange("(k p) n -> p k n", p=128)
        step = 512
        for c0 in range(0, ncols, step):
            for kh in range(2):
                S.dma(q, dst[:, kh * 4:(kh + 1) * 4, c0:c0 + step], v[:, kh * 4:(kh + 1) * 4, col0 + c0:col0 + c0 + step], writes=[dst])

    def xT_load(self, xin, t0, xs_, t):
        S = self.S
        xs = xs_[t % len(xs_)]
        S.dma("sp", xs[:], xin.t.ap()[t0 + t * 128:t0 + (t + 1) * 128, :], writes=[xs])

    def xT_tile(self, xs_, xb_, pT, xT, t):
        S, v, a, pe = self.S, self.v, self.a, self.pe
        xs = xs_[t % len(xs_)]
        xb = xb_[t % len(xb_)]
        S.op("dve", lambda: v.tensor_copy(out=xb[:], in_=xs[:]), reads=[xs], writes=[xb])
        p = pT[t % len(pT)]
        for k in range(8):
            S.op("pe", lambda: pe.transpose(out=p[:, k, :], in_=xb[:, k * 128:(k + 1) * 128], identity=self.ident[:]),
                 reads=[xb, self.ident], writes=[p])
        S.op("act", lambda: a.copy(out=xT[:, :, t * 128:(t + 1) * 128], in_=p[:]), reads=[p], writes=[xT])

    def xT_chunk(self, xin, t0, xs_, xb_, pT, xT, ntile=4):
        for t in range(ntile):
            self.xT_load(xin, t0, xs_, t)
            self.xT_tile(xs_, xb_, pT, xT, t)

    def conv_layer(self, jl, l, T, xin, xout, hook=None):
        S, v, g, a, pe = self.S, self.v, self.g, self.a, self.pe
        NCH = NTOK // 512
        with ExitStack() as es2:
            old = S.es
            S.es = es2
            win = S.sb("cv_win", [128, 8, 3 * D], BF16)
            self.load_w_bf16(win, T["conv_w_in"].t.ap()[jl], 3 * D)
            if hook is not None:
                hook()
            xs_ = [S.sb(f"cv_xs{i}", [128, D], F32) for i in range(4)]
            xb_ = [S.sb(f"cv_xb{i}", [128, D], BF16) for i in range(2)]
            pT = [S.ps(f"cv_pT{i}", [128, 8, 128], BF16) for i in range(2)]
            xT = [S.sb(f"cv_xT{i}", [128, 8, 512], BF16) for i in range(2)]
            ph = [[S.ps(f"cv_ph{i}_{p}", [128, 512], F32) for p in range(3)] for i in range(2)]
            gbs = [S.sb(f"cv_gbs{i}", [128, 512], F32) for i in range(2)]
            us = [S.sb(f"cv_us{i}", [128, 512], F32) for i in range(2)]
            zs = [S.sb(f"cv_zs{i}", [128, 512], F32) for i in range(2)]
            self.xT_chunk(xin, 0, xs_, xb_, pT, xT[0])
            for ch in range(NCH):
                xt = xT[ch % 2]
                nxt = ch + 1 < NCH
                if nxt:
                    for t in range(4):
                        self.xT_load(xin, (ch + 1) * 512, xs_, t)
                for c in range(8):
                    if nxt and c % 2 == 1:
                        self.xT_tile(xs_, xb_, pT, xT[(ch + 1) % 2], c // 2)
                    i = c % 2
                    for p in range(3):
                        f0 = p * D + c * 128
                        for k in range(8):
                            S.op("pe", lambda: pe.matmul(ph[i][p][:], lhsT=win[:, k, f0:f0 + 128], rhs=xt[:, k, :], start=(k == 0), stop=(k == 7)),
                                 reads=[win, xt], writes=[ph[i][p]])
                    S.op("act", lambda: a.copy(out=gbs[i][:], in_=ph[i][0][:]), reads=[ph[i][0]], writes=[gbs[i]])
                    S.op("act", lambda: a.copy(out=us[i][:], in_=ph[i][2][:]), reads=[ph[i][2]], writes=[us[i]])
                    S.op("dve", lambda: v.tensor_tensor(out=zs[i][:], in0=ph[i][1][:], in1=us[i][:], op=ALU.mult),
                         reads=[ph[i][1], us[i]], writes=[zs[i]])
                    S.dma("sp", T["cv_gb"].t.ap()[c * 128:(c + 1) * 128, ch * 512:(ch + 1) * 512], gbs[i][:], reads=[gbs[i]])
                    S.dma("sp", T["cv_z"].t.ap()[c * 128:(c + 1) * 128, ch * 512:(ch + 1) * 512], zs[i][:], reads=[zs[i]])
            S.barrier()
            S.es = old
        with ExitStack() as es2:
            old = S.es
            S.es = es2
            wout = S.sb("cv_wout", [128, 8, D], BF16)
            self.load_w_bf16(wout, T["conv_w_out"].t.ap()[jl], D)
            cw = S.sb("cv_cw", [128, 8, 3], F32)
            with self.nc.allow_non_contiguous_dma(reason="tiny conv weight transpose load"):
                for j3 in range(3):
                    S.dma("sp", cw[:, :, j3], T["conv_w"].t.ap()[jl, j3].rearrange("(c p) -> p c", p=128), writes=[cw])
            gt = S.sb("cv_g", [128, D], F32)
            bt = S.sb("cv_b", [128, D], F32)
            S.dma("sp", gt[:], T["ln_g"].t.ap()[l, 0:1, :].partition_broadcast(128), writes=[gt])
            S.dma("sp", bt[:], T["ln_b"].t.ap()[l, 0:1, :].partition_broadcast(128), writes=[bt])
            zt = [S.sb(f"cv_zt{i}", [128, 8, 514], F32) for i in range(2)]
            gbt = [S.sb(f"cv_gbt{i}", [128, 8, 512], F32) for i in range(2)]
            zc = [S.sb(f"cv_zc{i}", [128, 512], F32) for i in range(2)]
            gT = [S.sb(f"cv_gT{i}", [128, 8, 512], BF16) for i in range(2)]
            xr = [S.sb(f"cv_xr{i}", [128, D], F32) for i in range(2)]
            z1 = [S.sb(f"cv_z1{i}", [128, D], F32) for i in range(2)]
            xo = [S.sb(f"cv_xo{i}", [128, D], F32) for i in range(2)]
            py = [[S.ps(f"cv_py{i}_{n}", [128, 512], F32) for n in range(2)] for i in range(2)]
            st = [S.sb(f"cv_st{i}", [128, 2, 6], F32) for i in range(2)]
            mv = [S.sb(f"cv_mv{i}", [128, 2], F32) for i in range(2)]
            rstd = [S.sb(f"cv_rstd{i}", [128, 1], F32) for i in range(2)]
            pend = []
            R = self.router_alloc(l, T) if self.fuse_router else None
            rxb = [S.sb(f"cv_rxb{i}", [128, D], BF16) for i in range(2)]
            CPB = SEQ // 512
            for ch in range(NCH):
                i = ch % 2
                z_, gb_ = zt[i], gbt[i]
                t0 = ch * 512
                first = (ch % CPB == 0)
                last = (ch % CPB == CPB - 1)
                lo = 1 if first else 0
                hi = 513 if last else 514
                if first:
                    S.op("dve", lambda: v.memset(z_[:, :, 0:1], 0.0), writes=[z_])
                if last:
                    S.op("dve", lambda: v.memset(z_[:, :, 513:514], 0.0), writes=[z_])
                S.dma("sp", z_[:, :, lo:hi], T["cv_z"].t.ap()[:, t0 - 1 + lo:t0 - 1 + hi].rearrange("(c p) t -> p c t", p=128), writes=[z_])
                S.dma("sp", gb_[:], T["cv_gb"].t.ap()[:, t0:t0 + 512].rearrange("(c p) t -> p c t", p=128), writes=[gb_])
                g_ = gT[i]
                for c in range(8):
                    zz = zc[c % 2]
                    S.op("act", lambda: a.activation(out=zz[:], in_=z_[:, c, 0:512], func=AF.Copy, scale=cw[:, c, 0:1]),
                         reads=[z_, cw], writes=[zz])
                    S.op("dve", lambda: v.scalar_tensor_tensor(out=zz[:], in0=z_[:, c, 1:513], scalar=cw[:, c, 1:2], in1=zz[:], op0=ALU.mult, op1=ALU.add),
                         reads=[z_, cw, zz], writes=[zz])
                    S.op("dve", lambda: v.scalar_tensor_tensor(out=zz[:], in0=z_[:, c, 2:514], scalar=cw[:, c, 2:3], in1=zz[:], op0=ALU.mult, op1=ALU.add),
                         reads=[z_, cw, zz], writes=[zz])
                    S.op("dve", lambda: v.tensor_tensor(out=g_[:, c, :], in0=zz[:], in1=gb_[:, c, :], op=ALU.mult),
                         reads=[zz, gb_], writes=[g_])
                for t in range(4):
                    ti = (ch * 4 + t) % 2
                    tok0 = t0 + t * 128
                    S.dma("sp", xr[ti][:], xin.t.ap()[tok0:tok0 + 128, :], writes=[xr[ti]])
                    for n in range(2):
                        for c in range(8):
                            S.op("pe", lambda: pe.matmul(py[ti][n][:], lhsT=g_[:, c, t * 128:(t + 1) * 128], rhs=wout[:, c, n * 512:(n + 1) * 512],
                                                         start=(c == 0), stop=(c == 7)), reads=[g_, wout], writes=[py[ti][n]])
                        S.op("dve", lambda: v.scalar_tensor_tensor(out=z1[ti][:, n * 512:(n + 1) * 512], in0=xr[ti][:, n * 512:(n + 1) * 512], scalar=ALPHA,
                                                                   in1=py[ti][n][:], op0=ALU.mult, op1=ALU.add),
                             reads=[xr[ti], py[ti][n]], writes=[z1[ti]])
                    self.ln_a(z1[ti], st[ti], mv[ti], rstd[ti])

                    def fin(ti=ti, tok0=tok0, jt=ch * 4 + t):
                        self.ln_b(z1[ti], xo[ti], gt, bt, mv[ti], rstd[ti])
                        S.dma("sp", xout.t.ap()[tok0:tok0 + 128, :], xo[ti][:], reads=[xo[ti]])
                        if R is not None:
                            self.route_and_stage(jt, xo[ti], R, rxb[ti], T)
                    if pend:
                        pend.pop()()
                    pend.append(fin)
            pend.pop()()
            S.barrier()
            S.es = old

    def attn_layer(self, jl, l, T, xin, xout, hook=None):
        S, v, g, a, pe = self.S, self.v, self.g, self.a, self.pe
        NCH = NTOK // 512
        lambda_init = 0.8 - 0.6 * math.exp(-0.3 * l)
        with ExitStack() as es2:
            old = S.es
            S.es = es2
            win = S.sb("at_win", [128, 8, 3 * D], BF16)
            self.load_w_bf16(win, T["attn_w_in"].t.ap()[jl], 3 * D)
            if hook is not None:
                hook()
            xs_ = [S.sb(f"at_xs{i}", [128, D], F32) for i in range(4)]
            xb_ = [S.sb(f"at_xb{i}", [128, D], BF16) for i in range(2)]
            pT = [S.ps(f"at_pT{i}", [128, 8, 128], BF16) for i in range(2)]
            xT = [S.sb(f"at_xT{i}", [128, 8, 512], BF16) for i in range(2)]
            ph = [S.ps(f"at_ph{i}", [128, 512], F32) for i in range(4)]
            qs_ = [S.sb(f"at_qs{i}", [128, 512], BF16) for i in range(4)]
            vs_ = [S.sb(f"at_vs{i}", [128, D], BF16) for i in range(2)]
            cnt = 0
            self.xT_chunk(xin, 0, xs_, xb_, pT, xT[0])
            for ch in range(NCH):
                xt = xT[ch % 2]
                nxt = ch + 1 < NCH
                if nxt:
                    for t in range(4):
                        self.xT_load(xin, (ch + 1) * 512, xs_, t)
                for f in range(16):
                    if nxt and f % 4 == 3:
                        self.xT_tile(xs_, xb_, pT, xT[(ch + 1) % 2], f // 4)
                    i = cnt % 4
                    cnt += 1
                    for k in range(8):
                        S.op("pe", lambda: pe.matmul(ph[i][:], lhsT=win[:, k, f * 128:(f + 1) * 128], rhs=xt[:, k, :], start=(k == 0), stop=(k == 7)),
                             reads=[win, xt], writes=[ph[i]])
                    if f % 2 == 0:
                        S.op("act", lambda: a.copy(out=qs_[i][:], in_=ph[i][:]), reads=[ph[i]], writes=[qs_[i]])
                    else:
                        S.op("dve", lambda: v.tensor_copy(out=qs_[i][:], in_=ph[i][:]), reads=[ph[i]], writes=[qs_[i]])
                    S.dma("sp", T["at_qk"].t.ap()[f, :, ch * 512:(ch + 1) * 512], qs_[i][:], reads=[qs_[i]])
                for t in range(4):
                    vv = vs_[t % 2]
                    for n in range(2):
                        i = cnt % 4
                        cnt += 1
                        for k in range(8):
                            S.op("pe", lambda: pe.matmul(ph[i][:], lhsT=xt[:, k, t * 128:(t + 1) * 128], rhs=win[:, k, 2 * D + n * 512:2 * D + (n + 1) * 512],
                                                         start=(k == 0), stop=(k == 7)), reads=[win, xt], writes=[ph[i]])
                        if n == 0:
                            S.op("act", lambda: a.copy(out=vv[:, 0:512], in_=ph[i][:]), reads=[ph[i]], writes=[vv])
                        else:
                            S.op("dve", lambda: v.tensor_copy(out=vv[:, 512:1024], in_=ph[i][:]), reads=[ph[i]], writes=[vv])
                    tok0 = ch * 512 + t * 128
                    S.dma("sp", T["at_v"].t.ap()[tok0:tok0 + 128, :], vv[:], reads=[vv])
            S.barrier()
            S.es = old
        with ExitStack() as es2:
            old = S.es
            S.es = es2
            lamt = S.sb("at_lamt", [128, 256], F32)
            S.dma("sp", lamt[:], T["attn_lambda"].t.ap()[jl:jl + 1].rearrange("o a b -> o (a b)").partition_broadcast(128), writes=[lamt])
            lprod = S.sb("at_lprod", [128, 2, 64], F32)
            lsum = S.sb("at_lsum", [128, 2], F32)
            lv4 = lamt[:].rearrange("p (a b) -> p a b", a=4)
            S.op("dve", lambda: v.tensor_tensor(out=lprod[:, 0, :], in0=lv4[:, 0, :], in1=lv4[:, 1, :], op=ALU.mult), reads=[lamt], writes=[lprod])
            S.op("dve", lambda: v.tensor_tensor(out=lprod[:, 1, :], in0=lv4[:, 2, :], in1=lv4[:, 3, :], op=ALU.mult), reads=[lamt, lprod], writes=[lprod])
            S.op("dve", lambda: v.tensor_reduce(out=lsum[:], in_=lprod[:], axis=AX.X, op=ALU.add), reads=[lprod], writes=[lsum])
            S.op("act", lambda: a.activation(out=lsum[:], in_=lsum[:], func=AF.Exp), reads=[lsum], writes=[lsum])
            nlam = S.sb("at_nlam", [128, 1], F32)
            S.op("dve", lambda: v.tensor_tensor(out=nlam[:], in0=lsum[:, 1:2], in1=lsum[:, 0:1], op=ALU.subtract), reads=[lsum], writes=[nlam])
            S.op("dve", lambda: v.tensor_scalar(out=nlam[:], in0=nlam[:], scalar1=-lambda_init, scalar2=None, op0=ALU.add), reads=[nlam], writes=[nlam])
            gsub = S.sb("at_gsub", [128, 128], F32)
            S.dma("sp", gsub[:], T["attn_subln"].t.ap()[jl:jl + 1, :].partition_broadcast(128), writes=[gsub])
            S.op("dve", lambda: v.tensor_scalar(out=gsub[:], in0=gsub[:], scalar1=1.0 - lambda_init, scalar2=None, op0=ALU.mult), reads=[gsub], writes=[gsub])
            cb = S.sb("at_cb", [128, 16], F32)
            S.dma("sp", cb[:], T["cbias"].t.ap(), writes=[cb])
            zerob = S.sb("at_zero", [128, 512], BF16)
            S.op("dve", lambda: v.memset(zerob[:], 0.0), writes=[zerob])
            qT = [[S.sb(f"at_qT{i}_{m}", [128, SEQ], BF16) for m in range(2)] for i in range(2)]
            for i in range(2):
                S.op("dve", lambda: v.memset(qT[i][0][64:128, :], 0.0), writes=[qT[i][0]])
                S.op("dve", lambda: v.memset(qT[i][1][0:64, :], 0.0), writes=[qT[i][1]])
            kT = [S.sb(f"at_kT{i}", [128, SEQ], BF16) for i in range(2)]
            va = [S.sb(f"at_va{i}", [128, 32, 129], BF16) for i in range(2)]
            for i in range(2):
                S.op("dve", lambda: v.memset(va[i][:, :, 128:129], 1.0), writes=[va[i]])
            Tf = [S.sb(f"at_Tf{i}", [128, 1152], F32) for i in range(2)]
            Thi = [S.sb(f"at_Thi{i}", [128, 1152], BF16) for i in range(2)]
            Tlo = [S.sb(f"at_Tlo{i}", [128, 1152], BF16) for i in range(2)]
            Sps = [[S.ps(f"at_S{m}_{i}", [128, 512], F32) for i in range(2)] for m in range(2)]
            Ob = [S.ps(f"at_O{i}", [128, 512], F32) for i in range(3)]
            PT = [[S.sb(f"at_PT{m}_{i}", [128, 512], BF16) for i in range(3)] for m in range(2)]
            Osb = [S.sb(f"at_Osb{i}", [128, 8, 129], F32) for i in range(2)]
            rr = S.sb("at_rr", [128, 8], F32)
            o_ = [S.sb(f"at_o{i}", [128, 128], F32) for i in range(4)]
            sq = S.sb("at_sq", [128, 128], F32)
            ss = S.sb("at_ss", [128, 4], F32)
            aob = [S.sb(f"at_aob{i}", [128, 4, 128], BF16) for i in range(2)]

            def load_bh(ih):
                b, h = divmod(ih, 8)
                i = ih % 2
                S.dma("sp", qT[i][0][0:64, :], T["at_qk"].t.ap()[h, 0:64, b * SEQ:(b + 1) * SEQ], writes=[qT[i][0]])
                S.dma("sp", qT[i][1][64:128, :], T["at_qk"].t.ap()[h, 64:128, b * SEQ:(b + 1) * SEQ], writes=[qT[i][1]])
                S.dma("sp", kT[i][:], T["at_qk"].t.ap()[8 + h, :, b * SEQ:(b + 1) * SEQ], writes=[kT[i]])
                S.dma("sp", va[i][:, :, 0:128], T["at_v"].t.ap()[b * SEQ:(b + 1) * SEQ, h * 128:(h + 1) * 128].rearrange("(i p) c -> p i c", p=128),
                      writes=[va[i]])
                S.dma("sp", Tf[i][:], T["biasT"].t.ap()[h], writes=[Tf[i]])
                S.op("dve", lambda: v.tensor_scalar(out=Tf[i][:], in0=Tf[i][:], scalar1=8.0, scalar2=None, op0=ALU.mult), reads=[Tf[i]], writes=[Tf[i]])
                S.op("dve", lambda: v.tensor_copy(out=Thi[i][:], in_=Tf[i][:]), reads=[Tf[i]], writes=[Thi[i]])
                S.op("dve", lambda: v.tensor_tensor(out=Tf[i][:], in0=Tf[i][:], in1=Thi[i][:], op=ALU.subtract), reads=[Tf[i], Thi[i]], writes=[Tf[i]])
                S.op("dve", lambda: v.tensor_copy(out=Tlo[i][:], in_=Tf[i][:]), reads=[Tf[i]], writes=[Tlo[i]])

            NBH = 2 * 8
            load_bh(0)
            step = 0
            for ih in range(NBH):
                b, h = divmod(ih, 8)
                i = ih % 2
                if ih + 1 < NBH:
                    load_bh(ih + 1)
                q_, k_, v_, thi, tlo = qT[i], kT[i], va[i], Thi[i], Tlo[i]
                for j in range(8):
                    def qk(ki):
                        d = ki - 4 * j
                        near = (-1 <= d <= 4)
                        for m in range(2):
                            sp_ = Sps[m][ki % 2]
                            S.op("pe", lambda: pe.matmul(sp_[:], lhsT=k_[:, ki * 128:(ki + 1) * 128],
                                                         rhs=q_[m][:, j * 512:(j + 1) * 512], start=True, stop=not near),
                                 reads=[k_, q_[m]], writes=[sp_])
                        for m in range(2):
                            sp_ = Sps[m][ki % 2]
                            pt_ = PT[m][ki % 3]
                            if near:
                                cs = 512 - 128 * d
                                S.op("pe", lambda: pe.matmul(sp_[:], lhsT=self.ident[:], rhs=thi[:, cs:cs + 512], start=False, stop=True),
                                     reads=[self.ident, thi], writes=[sp_])
                                S.op("act", lambda: a.activation(out=pt_[:], in_=sp_[:], func=AF.Exp, scale=0.125), reads=[sp_], writes=[pt_])
                            else:
                                col = 2 * h + (1 if d > 0 else 0)
                                S.op("act", lambda: a.activation(out=pt_[:], in_=sp_[:], func=AF.Exp, bias=cb[:, col:col + 1], scale=0.125),
                                     reads=[sp_, cb], writes=[pt_])

                    def av(ki):
                        for m in range(2):
                            pt_ = PT[m][ki % 3]
                            for qs in range(4):
                                acc = m * 4 + qs
                                ob = Ob[acc // 3]
                                c0 = (acc % 3) * 129
                                S.op("pe", lambda: pe.matmul(ob[:, c0:c0 + 129], lhsT=pt_[:, qs * 128:(qs + 1) * 128], rhs=v_[:, ki, :],
                                                             start=False, stop=(ki == 31), skip_group_check=True),
                                     reads=[pt_, v_], writes=[ob])
                    qk(0)
                    for ob in Ob:
                        S.op("pe", lambda: pe.matmul(ob[:], lhsT=zerob[:, 0:128], rhs=zerob[:], start=True, stop=True), reads=[zerob], writes=[ob])
                    for ki in range(32):
                        if ki + 1 < 32:
                            qk(ki + 1)
                        av(ki)
                    os_ = Osb[step % 2]
                    ab_ = aob[step % 2]
                    step += 1
                    for bi in range(3):
                        na = 3 if bi < 2 else 2
                        eng, en = ("dve", v.tensor_copy) if bi != 1 else ("act", a.copy)
                        S.op(eng, lambda: en(out=os_[:, bi * 3:bi * 3 + na, :], in_=Ob[bi][:, 0:na * 129].rearrange("p (a c) -> p a c", c=129)),
                             reads=[Ob[bi]], writes=[os_])
                    S.op("dve", lambda: v.reciprocal(out=rr[:], in_=os_[:, :, 128]), reads=[os_], writes=[rr])
                    S.op("dve", lambda: v.tensor_scalar(out=rr[:, 4:8], in0=rr[:, 4:8], scalar1=nlam[:, 0:1], scalar2=None, op0=ALU.mult),
                         reads=[rr, nlam], writes=[rr])
                    for qs in range(4):
                        S.op("dve", lambda: v.tensor_scalar(out=o_[qs][:], in0=os_[:, qs, 0:128], scalar1=rr[:, qs:qs + 1], scalar2=None, op0=ALU.mult),
                             reads=[os_, rr], writes=[o_[qs]])
                        S.op("dve", lambda: v.scalar_tensor_tensor(out=o_[qs][:], in0=os_[:, 4 + qs, 0:128], scalar=rr[:, 4 + qs:5 + qs], in1=o_[qs][:],
                                                                   op0=ALU.mult, op1=ALU.add), reads=[os_, rr, o_[qs]], writes=[o_[qs]])
                        S.op("dve", lambda: v.tensor_tensor(out=sq[:], in0=o_[qs][:], in1=o_[qs][:], op=ALU.mult), reads=[o_[qs]], writes=[sq])
                        S.op("dve", lambda: v.reduce_sum(out=ss[:, qs:qs + 1], in_=sq[:], axis=AX.X), reads=[sq], writes=[ss])
                    S.op("dve", lambda: v.tensor_scalar(out=ss[:], in0=ss[:], scalar1=1.0 / 128.0, scalar2=LN_EPS, op0=ALU.mult, op1=ALU.add),
                         reads=[ss], writes=[ss])
                    S.op("act", lambda: a.sqrt(out=ss[:], in_=ss[:]), reads=[ss], writes=[ss])
                    S.op("dve", lambda: v.reciprocal(out=ss[:], in_=ss[:]), reads=[ss], writes=[ss])
                    for qs in range(4):
                        S.op("dve", lambda: v.scalar_tensor_tensor(out=ab_[:, qs, :], in0=o_[qs][:], scalar=ss[:, qs:qs + 1], in1=gsub[:],
                                                                   op0=ALU.mult, op1=ALU.mult), reads=[o_[qs], ss, gsub], writes=[ab_])
                    tok0 = b * SEQ + j * 512
                    S.dma("sp", T["at_ao"].t.ap()[tok0:tok0 + 512, h * 128:(h + 1) * 128].rearrange("(q p) c -> p q c", p=128), ab_[:], reads=[ab_])
            S.barrier()
            S.es = old
        with ExitStack() as es2:
            old = S.es
            S.es = es2
            wout = S.sb("at_wout", [128, 8, D], BF16)
            self.load_w_bf16(wout, T["attn_w_out"].t.ap()[jl], D)
            gt = S.sb("at_g", [128, D], F32)
            bt = S.sb("at_b", [128, D], F32)
            S.dma("sp", gt[:], T["ln_g"].t.ap()[l, 0:1, :].partition_broadcast(128), writes=[gt])
            S.dma("sp", bt[:], T["ln_b"].t.ap()[l, 0:1, :].partition_broadcast(128), writes=[bt])
            ao = [S.sb(f"at_ao{i}", [128, D], BF16) for i in range(3)]
            xr = [S.sb(f"at_xr{i}", [128, D], F32) for i in range(3)]
            pT = [S.ps(f"at_pTc{i}", [128, 8, 128], BF16) for i in range(1)] * 2
            R = self.router_alloc(l, T) if self.fuse_router else None
            rxb = [S.sb(f"at_rxb{i}", [128, D], BF16) for i in range(2)]
            aoT = [S.sb(f"at_aoT{i}", [128, 8, 128], BF16) for i in range(2)]
            py = [[S.ps(f"at_py{i}_{n}", [128, 512], F32) for n in range(2)] for i in range(2)]
            z1 = [S.sb(f"at_z1{i}", [128, D], F32) for i in range(2)]
            xo = [S.sb(f"at_xo{i}", [128, D], F32) for i in range(2)]
            st = [S.sb(f"at_st{i}", [128, 2, 6], F32) for i in range(2)]
            mv = [S.sb(f"at_mv{i}", [128, 2], F32) for i in range(2)]
            rstd = [S.sb(f"at_rstd{i}", [128, 1], F32) for i in range(2)]
            pend = []

            def ld(t):
                S.dma("sp", ao[t % 3][:], T["at_ao"].t.ap()[t * 128:(t + 1) * 128, :], writes=[ao[t % 3]])
                S.dma("sp", xr[t % 3][:], xin.t.ap()[t * 128:(t + 1) * 128, :], writes=[xr[t % 3]])
            ld(0)
            ld(1)
            for t in range(NT):
                if t + 2 < NT:
                    ld(t + 2)
                i = t % 2
                for c in range(8):
                    S.op("pe", lambda: pe.transpose(out=pT[i][:, c, :], in_=ao[t % 3][:, c * 128:(c + 1) * 128], identity=self.ident[:]),
                         reads=[ao[t % 3], self.ident], writes=[pT[i]])
                S.op("act", lambda: a.copy(out=aoT[i][:], in_=pT[i][:]), reads=[pT[i]], writes=[aoT[i]])
                for n in range(2):
                    for c in range(8):
                        S.op("pe", lambda: pe.matmul(py[i][n][:], lhsT=aoT[i][:, c, :], rhs=wout[:, c, n * 512:(n + 1) * 512], start=(c == 0), stop=(c == 7)),
                             reads=[aoT[i], wout], writes=[py[i][n]])
                    S.op("dve", lambda: v.scalar_tensor_tensor(out=z1[i][:, n * 512:(n + 1) * 512], in0=xr[t % 3][:, n * 512:(n + 1) * 512], scalar=ALPHA,
                                                               in1=py[i][n][:], op0=ALU.mult, op1=ALU.add), reads=[xr[t % 3], py[i][n]], writes=[z1[i]])
                self.ln_a(z1[i], st[i], mv[i], rstd[i])

                def fin(i=i, t=t):
                    self.ln_b(z1[i], xo[i], gt, bt, mv[i], rstd[i])
                    S.dma("sp", xout.t.ap()[t * 128:(t + 1) * 128, :], xo[i][:], reads=[xo[i]])
                    if R is not None:
                        self.route_and_stage(t, xo[i], R, rxb[i], T)
                if pend:
                    pend.pop()()
                pend.append(fin)
            pend.pop()()
            S.barrier()
            S.es = old


def _rel_bucket_np(rel):
    try:
        import jax
        import jax.numpy as jnp
        cpu = jax.devices("cpu")[0]
        with jax.default_device(cpu):
            r = jnp.asarray(rel, dtype=jnp.int32)
            nb = 16
            max_exact = 8
            ret = jnp.where(r > 0, nb, 0)
            n = jnp.abs(r)
            nf = jnp.maximum(n, 1).astype(jnp.float32)
            large = max_exact + (jnp.log(nf / max_exact) / math.log(128 / max_exact) * (nb - max_exact)).astype(jnp.int32)
            large = jnp.minimum(large, nb - 1)
            return np.asarray(ret + jnp.where(n < max_exact, n, large))
    except Exception:
        rel = np.asarray(rel, dtype=np.int32)
        nb, max_exact = 16, 8
        ret = np.where(rel > 0, nb, 0)
        n = np.abs(rel)
        nf = np.maximum(n, 1).astype(np.float32)
        large = max_exact + (np.log(nf / np.float32(max_exact)) / np.float32(math.log(128 / max_exact)) * np.float32(nb - max_exact)).astype(np.int32)
        large = np.minimum(large, nb - 1)
        return ret + np.where(n < max_exact, n, large)


def bias_tables(rel_bias):
    kk = np.arange(128)[:, None]
    c = np.arange(1152)[None, :]
    bucket = _rel_bucket_np(kk - c + 512)
    biasT = np.ascontiguousarray(np.transpose(rel_bias[bucket], (2, 0, 1))).astype(np.float32)
    cb = np.stack([rel_bias[15], rel_bias[31]], axis=1).reshape(-1)
    cbias = np.ascontiguousarray(np.broadcast_to(cb[None, :], (128, 16))).astype(np.float32)
    return biasT, cbias


def build_program():
    nc = bass.Bass("TRN2", target_bir_lowering=False)
    with ExitStack() as es:
        k = K(nc, es)
        k.nl = DEPTH
        k.fuse_router = False
        S = k.S
        T = {}
        x = S.dram("x", [NTOK, D], F32, kind="ExternalInput")
        for nm, shp in (("attn_w_in", [2, D, 3 * D]), ("attn_w_out", [2, D, D]), ("attn_lambda", [2, 4, 64]), ("attn_subln", [2, 128]),
                        ("biasT", [8, 128, 1152]), ("cbias", [128, 16]),
                        ("conv_w_in", [2, D, 3 * D]), ("conv_w", [2, 3, D]), ("conv_w_out", [2, D, D]),
                        ("router_w", [DEPTH, D, E]), ("router_b", [DEPTH, E]),
                        ("w1", [DEPTH * E * D, 2 * D]), ("b1", [DEPTH * E, 2 * D]), ("w2", [DEPTH * E * D, D]), ("b2", [DEPTH * E, D]),
                        ("ln_g", [DEPTH, 2, D]), ("ln_b", [DEPTH, 2, D])):
            T[nm] = S.dram(nm, shp, F32, kind="ExternalInput")
        out = S.dram("out", [NTOK, D], F32, kind="ExternalOutput")
        T["x1"] = S.dram("x1", [NTOK, D], F32)
        T["x1b"] = S.dram("x1b", [NTOK, D], BF16)
        T["xs"] = S.dram("xs", [PROWS, D], BF16)
        T["ys"] = S.dram("ys", [PROWS, D], BF16)
        T["cv_gb"] = S.dram("cv_gb", [D, NTOK], F32)
        T["cv_z"] = S.dram("cv_z", [D, NTOK], F32)
        T["at_qk"] = S.dram("at_qk", [16, 128, NTOK], BF16)
        T["at_v"] = S.dram("at_v", [NTOK, D], BF16)
        T["at_ao"] = S.dram("at_ao", [NTOK, D], BF16)
        T["w1c"] = S.dram("w1c", [E * 128, 8 * 2 * D], BF16)
        T["w2c"] = S.dram("w2c", [E * 128, 8 * D], BF16)
        T["b1c"] = S.dram("b1c", [E, 2 * D], BF16)
        T["b2c"] = S.dram("b2c", [E, D], BF16)
        xres = S.dram("xres", [NTOK, D], F32)
        k.consts()
        with ExitStack() as es2:
            old = S.es
            S.es = es2
            zt = S.sb("zfill", [128, 8 * D], BF16)
            S.op("pool", lambda: k.g.memset(zt[:], 0.0), writes=[zt])
            rows_per = 128 * 8
            for r0 in range(0, PROWS, rows_per):
                S.dma("sp", T["xs"].t.ap()[r0:r0 + rows_per, :].rearrange("(p a) d -> p (a d)", p=128), zt[:], reads=[zt])
            S.barrier()
            S.es = old
        for l in range(DEPTH):
            xin = x if l == 0 else xres
            xo = out if l == DEPTH - 1 else xres
            hook = (lambda l=l: k.convert_weights(l, T))
            if l % 2 == 0:
                k.attn_layer(l // 2, l, T, xin, T["x1"], hook=hook)
            else:
                k.conv_layer(l // 2, l, T, xin, T["x1"], hook=hook)
            k.moe_layer(l, T, xo)
        S.barrier()
    return nc


_NC_CACHE = {}


def kernel(x, rel_bias, attn_w_in, attn_lambda, attn_subln, attn_w_out, conv_w_in, conv_w, conv_w_out,
           router_w, router_b, w1, b1, w2, b2, ln_g, ln_b):
    f32 = lambda a_: np.ascontiguousarray(np.asarray(a_, dtype=np.float32))
    x = f32(x)
    B = x.shape[0]
    ncores = 8
    per = B // ncores
    biasT, cbias = bias_tables(f32(rel_bias))
    shared = {
        "attn_w_in": f32(attn_w_in), "attn_w_out": f32(attn_w_out), "attn_lambda": f32(attn_lambda), "attn_subln": f32(attn_subln),
        "biasT": biasT, "cbias": cbias,
        "conv_w_in": f32(conv_w_in), "conv_w": f32(conv_w), "conv_w_out": f32(conv_w_out),
        "router_w": f32(router_w), "router_b": f32(router_b),
        "w1": f32(w1).reshape(DEPTH * E * D, 2 * D), "b1": f32(b1).reshape(DEPTH * E, 2 * D),
        "w2": f32(w2).reshape(DEPTH * E * D, D), "b2": f32(b2).reshape(DEPTH * E, D),
        "ln_g": f32(ln_g), "ln_b": f32(ln_b),
    }
    if "nc" not in _NC_CACHE:
        _NC_CACHE["nc"] = build_program()
    nc = _NC_CACHE["nc"]
    in_maps = []
    for c in range(ncores):
        m = dict(shared)
        m["x"] = x[c * per:(c + 1) * per].reshape(NTOK, D)
        in_maps.append(m)
    res = run_bass_kernel_spmd(nc, in_maps, core_ids=list(range(ncores)))
    outs = [np.asarray(r["out"]).reshape(per, SEQ, D) for r in res.results]
    return np.concatenate(outs, axis=0).astype(np.float32)
```
